# Optimizing a Trainium2 kernel written in Bass

```python
import math
import jax
import jax.numpy as jnp
from jax import lax
import numpy as np

D_MODEL = 1024
BATCH = 1
SEQ = 16384
DEPTH = 2
DEC_BATCH = 32
DEC_SEQ = 2048
PAST_LEN = 128

GRID_W = 64
HEAD_DIM = 64
EPS = 1e-6

A_HEADS = D_MODEL // (2 * HEAD_DIM)
A_KV_HEADS = 2
A_GROUP = A_HEADS // A_KV_HEADS
ROPE_THETA = 10000.0
Q_BLOCK = 128

DILATED_PAIRS = ((128, 1), (512, 4), (2048, 16))
B_GROUPS = len(DILATED_PAIRS)
B_HEADS = D_MODEL // (4 * HEAD_DIM)

C_HEADS = D_MODEL // (4 * HEAD_DIM)
NA_ROWS = 8
NA_COLS = 16

N_BUCKETS = 32
MAX_DISTANCE = 2048

N_EXPERTS = 16
EC_CAPACITY = 2
D_EXPERT = 2 * D_MODEL

A_Q = A_HEADS * HEAD_DIM
A_KV = A_KV_HEADS * HEAD_DIM
B_W = B_GROUPS * B_HEADS * HEAD_DIM
B_OUT = B_HEADS * HEAD_DIM
C_W = C_HEADS * HEAD_DIM
IN_SPLITS = (A_Q, A_KV, A_KV, B_W, B_W, B_W, C_W, C_W, C_W)
D_IN = sum(IN_SPLITS)
D_MIX = A_Q + B_OUT + C_W

kernel_name = "hybrid_bidir_encoder_gqa_dilated_na_ecmoe"


def rms_norm(x, gain):
    xf = x.astype(jnp.float32)
    y = xf * lax.rsqrt(jnp.mean(xf * xf, axis=-1, keepdims=True) + EPS)
    return (y * gain.astype(jnp.float32)).astype(x.dtype)


def axial_rope_tables(S):
    t = jnp.arange(S)
    row = (t // GRID_W).astype(jnp.float32)
    col = (t % GRID_W).astype(jnp.float32)
    half = HEAD_DIM // 2
    freqs = ROPE_THETA ** (-jnp.arange(0, half, 2, dtype=jnp.float32) / half)
    ang = jnp.concatenate([row[:, None] * freqs, col[:, None] * freqs], axis=-1)
    return jnp.cos(ang), jnp.sin(ang)


def apply_rope(x, cos, sin):
    xf = x.astype(jnp.float32).reshape(x.shape[:-1] + (HEAD_DIM // 2, 2))
    x1, x2 = xf[..., 0], xf[..., 1]
    c = cos[None, :, None, :]
    s = sin[None, :, None, :]
    out = jnp.stack([x1 * c - x2 * s, x1 * s + x2 * c], axis=-1).reshape(x.shape)
    return out.astype(x.dtype)


def t5_bucket(rel):
    nb = N_BUCKETS // 2
    max_exact = nb // 2
    ret = (rel > 0).astype(jnp.int32) * nb
    n = jnp.abs(rel)
    large = max_exact + (jnp.log(jnp.maximum(n, 1).astype(jnp.float32) / max_exact)
                         / math.log(MAX_DISTANCE / max_exact) * (nb - max_exact)).astype(jnp.int32)
    large = jnp.minimum(large, nb - 1)
    return ret + jnp.where(n < max_exact, n, large)


def mixer_a(q, k, v, q_gain, k_gain):
    B, S = q.shape[:2]
    cos, sin = axial_rope_tables(S)
    q = apply_rope(rms_norm(q, q_gain), cos, sin)
    k = apply_rope(rms_norm(k, k_gain), cos, sin)
    nblk = S // Q_BLOCK
    qb = q.reshape(B, nblk, Q_BLOCK, A_KV_HEADS, A_GROUP, HEAD_DIM).transpose(1, 0, 2, 3, 4, 5)
    scale = HEAD_DIM ** -0.5

    def block(qi):
        s = jnp.einsum('bqkgd,bskd->bkgqs', qi, k, preferred_element_type=jnp.float32) * scale
        p = jax.nn.softmax(s, axis=-1)
        return jnp.einsum('bkgqs,bskd->bqkgd', p.astype(v.dtype), v)

    o = lax.map(block, qb)
    return o.transpose(1, 0, 2, 3, 4, 5).reshape(B, S, A_Q)


def dilated_branch(q, k, v, bias_table, window, dilation):
    B, S, H, D = q.shape
    R = window // (2 * dilation)
    L = S // dilation
    nb = -(-L // R)
    Lp = nb * R

    def to_sub(x):
        return x.reshape(B, L, dilation, H, D).transpose(0, 2, 1, 3, 4)

    qs = jnp.pad(to_sub(q), ((0, 0), (0, 0), (0, Lp - L), (0, 0), (0, 0))).reshape(B, dilation, nb, R, H, D)

    def windows(x):
        xp = jnp.pad(to_sub(x), ((0, 0), (0, 0), (R, Lp - L + R), (0, 0), (0, 0))).reshape(B, dilation, nb + 2, R, H, D)
        return jnp.concatenate([xp[:, :, :-2], xp[:, :, 1:-1], xp[:, :, 2:]], axis=3)

    kw = windows(k)
    vw = windows(v)
    off = jnp.arange(3 * R)[None, :] - R - jnp.arange(R)[:, None]
    bias = bias_table[t5_bucket(off * dilation)].transpose(2, 0, 1)
    key_sub = jnp.arange(nb)[:, None] * R - R + jnp.arange(3 * R)[None, :]
    valid = ((jnp.abs(off) <= R)[None] & (key_sub[:, None, :] >= 0) & (key_sub[:, None, :] < L))
    s = jnp.einsum('brnqhd,brnkhd->brnhqk', qs, kw, preferred_element_type=jnp.float32) * (HEAD_DIM ** -0.5)
    s = jnp.where(valid[None, None, :, None], s + bias[None, None, None], -jnp.inf)
    m = jnp.max(s, axis=-1, keepdims=True)
    p = jnp.exp(s - m)
    den = jnp.sum(p, axis=-1)
    o = jnp.einsum('brnhqk,brnkhd->brnqhd', p.astype(vw.dtype), vw).astype(jnp.float32)
    o = o / den.transpose(0, 1, 2, 4, 3)[..., None]
    lse = (m[..., 0] + jnp.log(den)).transpose(0, 1, 2, 4, 3)
    o = o.reshape(B, dilation, Lp, H, D)[:, :, :L].transpose(0, 2, 1, 3, 4).reshape(B, S, H, D)
    lse = lse.reshape(B, dilation, Lp, H)[:, :, :L].transpose(0, 2, 1, 3).reshape(B, S, H)
    return o, lse


def mixer_b(q, k, v, rel_bias):
    B, S = q.shape[:2]
    outs, lses = [], []
    for g, (window, dilation) in enumerate(DILATED_PAIRS):
        o, lse = dilated_branch(q[:, :, g], k[:, :, g], v[:, :, g],
                                rel_bias[:, g * B_HEADS:(g + 1) * B_HEADS], window, dilation)
        outs.append(o)
        lses.append(lse)
    w = jax.nn.softmax(jnp.stack(lses, axis=0), axis=0)
    o = jnp.sum(w[..., None] * jnp.stack(outs, axis=0), axis=0)
    return o.reshape(B, S, B_OUT).astype(q.dtype)


def mixer_c(q, k, v, rpb):
    B, S, H, D = q.shape
    rows = S // GRID_W
    kr = min(NA_ROWS, rows)
    qg = q.reshape(B, rows, GRID_W, H, D)
    kg = k.reshape(B, rows, GRID_W, H, D)
    vg = v.reshape(B, rows, GRID_W, H, D)
    r = jnp.arange(rows)
    rs = jnp.clip(r - kr // 2, 0, rows - kr)
    row_idx = rs[:, None] + jnp.arange(kr)[None, :]
    kwin = kg[:, row_idx]
    vwin = vg[:, row_idx]
    c = jnp.arange(GRID_W)
    cs = jnp.clip(c - NA_COLS // 2, 0, GRID_W - NA_COLS)
    col_ok = (c[None, :] >= cs[:, None]) & (c[None, :] < cs[:, None] + NA_COLS)
    drow = row_idx - r[:, None]
    dcol = jnp.clip(c[None, :] - c[:, None], -(NA_COLS - 1), NA_COLS - 1)
    bias = rpb[:, drow[:, None, :, None] + NA_ROWS - 1, dcol[None, :, None, :] + NA_COLS - 1]
    s = jnp.einsum('brqhd,brkwhd->bhrqkw', qg, kwin, preferred_element_type=jnp.float32) * (HEAD_DIM ** -0.5)
    s = jnp.where(col_ok[:, None, :], s + bias[None].astype(jnp.float32), -jnp.inf)
    p = jax.nn.softmax(s.reshape(s.shape[:4] + (kr * GRID_W,)), axis=-1).reshape(s.shape)
    o = jnp.einsum('bhrqkw,brkwhd->brqhd', p.astype(vwin.dtype), vwin)
    return o.reshape(B, S, C_W)


def expert_choice_ffn(h, w_router, w_gate, w_up, w_down):
    B, S, Dm = h.shape
    n_tok = B * S
    cap = EC_CAPACITY * n_tok // N_EXPERTS
    tok = h.reshape(n_tok, Dm)
    aff = jax.nn.softmax(jnp.dot(tok.astype(jnp.float32), w_router.astype(jnp.float32)), axis=-1)
    gate, idx = lax.top_k(aff.T, cap)

    def expert(args):
        idx_e, gate_e, wg, wu, wd = args
        xe = tok[idx_e]
        hid = jax.nn.silu(xe @ wg) * (xe @ wu)
        return (hid @ wd) * gate_e[:, None].astype(h.dtype)

    ye = lax.map(expert, (idx, gate, w_gate, w_up, w_down))
    out = jnp.zeros((n_tok, Dm), h.dtype).at[idx.reshape(-1)].add(ye.reshape(-1, Dm))
    return out.reshape(B, S, Dm)


def encoder_layer(x, w_in, w_out, norm_mix, norm_ffn, q_gain, k_gain, out_gain,
                  na_rpb, rel_bias, w_router, w_gate, w_up, w_down):
    B, S, _ = x.shape
    h = rms_norm(x, norm_mix)
    proj = jnp.einsum('bsd,de->bse', h, w_in)
    a_q, a_k, a_v, b_q, b_k, b_v, c_q, c_k, c_v = jnp.split(
        proj, np.cumsum(IN_SPLITS)[:-1].tolist(), axis=-1)
    o_a = mixer_a(a_q.reshape(B, S, A_HEADS, HEAD_DIM), a_k.reshape(B, S, A_KV_HEADS, HEAD_DIM),
                  a_v.reshape(B, S, A_KV_HEADS, HEAD_DIM), q_gain, k_gain)
    bshape = (B, S, B_GROUPS, B_HEADS, HEAD_DIM)
    o_b = mixer_b(b_q.reshape(bshape), b_k.reshape(bshape), b_v.reshape(bshape), rel_bias)
    cshape = (B, S, C_HEADS, HEAD_DIM)
    o_c = mixer_c(c_q.reshape(cshape), c_k.reshape(cshape), c_v.reshape(cshape), na_rpb)
    g_a, g_b, g_c = jnp.split(out_gain, [A_Q, A_Q + B_OUT])
    mixed = jnp.concatenate([rms_norm(o_a, g_a), rms_norm(o_b, g_b), rms_norm(o_c, g_c)], axis=-1)
    x = x + jnp.einsum('bse,ed->bsd', mixed, w_out)
    x = x + expert_choice_ffn(rms_norm(x, norm_ffn), w_router, w_gate, w_up, w_down)
    return x


def run_trunk(x, w_in, w_out, norm_mix, norm_ffn, q_gain, k_gain, out_gain, na_rpb,
              rel_bias, w_router, w_gate, w_up, w_down, final_norm):
    for l in range(DEPTH):
        x = encoder_layer(x, w_in[l], w_out[l], norm_mix[l], norm_ffn[l], q_gain[l], k_gain[l],
                          out_gain[l], na_rpb[l], rel_bias, w_router[l], w_gate[l], w_up[l], w_down[l])
    return rms_norm(x, final_norm)


def setup_inputs(seed: int = 0) -> dict:
    key = jax.random.key(seed)
    ks = jax.random.split(key, 16)

    def nrm(k, shape, scale):
        return jax.random.normal(k, shape, jnp.float32) * scale

    return {
        "x_prompt": nrm(ks[0], (BATCH, SEQ, D_MODEL), 1.0),
        "x_sample": nrm(ks[1], (DEC_BATCH, DEC_SEQ, D_MODEL), 1.0),
        "w_in": nrm(ks[2], (DEPTH, D_MODEL, D_IN), D_MODEL ** -0.5),
        "w_out": nrm(ks[3], (DEPTH, D_MIX, D_MODEL), D_MIX ** -0.5),
        "norm_mix": 1.0 + nrm(ks[4], (DEPTH, D_MODEL), 0.01),
        "norm_ffn": 1.0 + nrm(ks[5], (DEPTH, D_MODEL), 0.01),
        "q_gain": 1.0 + nrm(ks[6], (DEPTH, HEAD_DIM), 0.01),
        "k_gain": 1.0 + nrm(ks[7], (DEPTH, HEAD_DIM), 0.01),
        "out_gain": 1.0 + nrm(ks[8], (DEPTH, D_MIX), 0.01),
        "na_rpb": nrm(ks[9], (DEPTH, C_HEADS, 2 * NA_ROWS - 1, 2 * NA_COLS - 1), 0.1),
        "rel_bias": nrm(ks[10], (N_BUCKETS, B_GROUPS * B_HEADS), 0.1),
        "w_router": nrm(ks[11], (DEPTH, D_MODEL, N_EXPERTS), D_MODEL ** -0.5),
        "w_gate": nrm(ks[12], (DEPTH, N_EXPERTS, D_MODEL, D_EXPERT), D_MODEL ** -0.5),
        "w_up": nrm(ks[13], (DEPTH, N_EXPERTS, D_MODEL, D_EXPERT), D_MODEL ** -0.5),
        "w_down": nrm(ks[14], (DEPTH, N_EXPERTS, D_EXPERT, D_MODEL), D_EXPERT ** -0.5),
        "final_norm": 1.0 + nrm(ks[15], (D_MODEL,), 0.01),
    }


def reference(x_prompt, x_sample, w_in, w_out, norm_mix, norm_ffn, q_gain, k_gain, out_gain,
              na_rpb, rel_bias, w_router, w_gate, w_up, w_down, final_norm):
    y_prompt = run_trunk(x_prompt, w_in, w_out, norm_mix, norm_ffn, q_gain, k_gain, out_gain,
                         na_rpb, rel_bias, w_router, w_gate, w_up, w_down, final_norm)
    y_sample = run_trunk(x_sample, w_in, w_out, norm_mix, norm_ffn, q_gain, k_gain, out_gain,
                         na_rpb, rel_bias, w_router, w_gate, w_up, w_down, final_norm)
    return (y_prompt, y_sample)
```

```python
import numpy as np
import ml_dtypes
import concourse.bass as bass
import concourse.mybir as mybir
from concourse.bass_utils import run_bass_kernel_spmd

F32 = mybir.dt.float32
BF16 = mybir.dt.bfloat16
I32 = mybir.dt.int32
ALU = mybir.AluOpType
AF = mybir.ActivationFunctionType
AX = mybir.AxisListType
NPBF = ml_dtypes.bfloat16

NCORES = 8
D = 1024
DEPTH = 2
SEQ_P = 16384
NSEQ_S = 32
SEQ_S = 2048
SEG = 2048
NSEG = 5
NTOK = NSEG * SEG
NT = NTOK // 128
HD = 64
D_IN = 3840
EPS = 1e-6
NEXP = 16
DEXP = 2048
NEG = -30000.0


class Buf:
    def __init__(self, ap):
        self.ap = ap
        self.w = None
        self.r = []

    def __getitem__(self, k):
        return self.ap[k]


class KB:
    R = 4
    NDMA = 24

    def __init__(self):
        self.nc = bass.Bass("TRN2", target_bir_lowering=False)
        nc = self.nc
        self.eng = {"pe": nc.tensor, "act": nc.scalar, "dve": nc.vector,
                    "pool": nc.gpsimd, "sp": nc.sync}
        self.sems = {e: [nc.alloc_semaphore(f"s_{e}{i}") for i in range(self.R)]
                     for e in ("pe", "act", "dve", "pool")}
        self.cnt = {e: 0 for e in ("pe", "act", "dve", "pool")}
        self.dsem = [nc.alloc_semaphore(f"s_dma{i}") for i in range(self.NDMA)]
        self.dval = [0] * self.NDMA
        self.dnext = 0
        self.waited = {}
        self.out_tokens = []
        self.nbuf = 0

    def sb(self, shape, dt, name=None):
        self.nbuf += 1
        return Buf(self.nc.alloc_sbuf_tensor(name or f"sb{self.nbuf}", list(shape), dt).ap())

    def ps(self, shape, dt=F32, name=None):
        self.nbuf += 1
        return Buf(self.nc.alloc_psum_tensor(name or f"ps{self.nbuf}", list(shape), dt).ap())

    def dram(self, name, shape, dt, kind):
        return self.nc.dram_tensor(name, list(shape), dt, kind=kind).ap()

    def _wait(self, waiter, tok):
        if tok is None:
            return
        key, n = tok
        if self.waited.get((waiter, key), 0) >= n:
            return
        self.waited[(waiter, key)] = n
        e = self.eng[waiter]
        if key[0] == "dma":
            e.wait_ge(self.dsem[key[1]], n * 16)
        else:
            e.wait_ge(self.sems[key[0]][(n - 1) % self.R], (n - 1) // self.R + 1)

    def _deps(self, waiter, reads, writes):
        for b in reads:
            self._wait(waiter, b.w)
        for b in writes:
            self._wait(waiter, b.w)
            for t in b.r:
                self._wait(waiter, t)

    def _commit(self, tok, reads, writes):
        for b in reads:
            b.r.append(tok)
            if len(b.r) > 64:
                b.r = b.r[-64:]
        for b in writes:
            b.w = tok
            b.r = []

    def op(self, e, fn, reads=(), writes=()):
        self._deps(e, reads, writes)
        ins = fn(self.eng[e])
        self.cnt[e] += 1
        n = self.cnt[e]
        ins.then_inc(self.sems[e][(n - 1) % self.R], 1)
        tok = ((e,), n)
        self._commit(tok, reads, writes)
        return tok

    def dma(self, q, out, in_, reads=(), writes=(), is_output=False, **kw):
        self._deps(q, reads, writes)
        i = self.dnext
        self.dnext = (self.dnext + 1) % self.NDMA
        if self.dval[i] > 0:
            self._wait(q, (("dma", i), self.dval[i]))
        self.dval[i] += 1
        self.eng[q].dma_start(out=out, in_=in_, **kw).then_inc(self.dsem[i], 16)
        tok = (("dma", i), self.dval[i])
        self._commit(tok, reads, writes)
        if is_output:
            self.out_tokens.append(tok)
        return tok

    def finish(self):
        for t in self.out_tokens:
            self._wait("sp", t)
        return self.nc


def _run(kb, in_maps):
    nc = kb.finish()
    res = run_bass_kernel_spmd(nc, in_maps, core_ids=list(range(NCORES)))
    return res.results


def build_l1():
    kb = KB()
    nc = kb.nc
    xT = kb.dram("xT", [D, NTOK], F32, "ExternalInput")
    w_in = kb.dram("w_in", [D, D_IN], F32, "ExternalInput")
    gmix = kb.dram("gmix", [128, 8], F32, "ExternalInput")
    gqk = kb.dram("gqk", [128, 640], F32, "ExternalInput")
    cosd = kb.dram("cos", [NTOK, 32], F32, "ExternalInput")
    sind = kb.dram("sin", [NTOK, 32], F32, "ExternalInput")
    proj = kb.dram("proj", [NTOK, D_IN], BF16, "ExternalOutput")

    gW = kb.sb([128, 8, D_IN], BF16, "gW")
    gm = kb.sb([128, 8], F32, "gm")
    gq = kb.sb([128, 640], F32, "gq")
    ones = kb.sb([128, 1], BF16, "ones")
    kb.dma("sp", gm.ap, gmix, writes=[gm])
    kb.dma("sp", gq.ap, gqk, writes=[gq])
    kb.op("pool", lambda e: e.memset(ones.ap, 1.0), writes=[ones])
    epsb = kb.sb([128, 1], F32, "epsb")
    kb.op("pool", lambda e: e.memset(epsb.ap, EPS), writes=[epsb])
    wst = [kb.sb([128, 1920], F32, f"wst{i}") for i in range(2)]
    w_v = w_in.rearrange("(c p) n -> p c n", p=128)
    k = 0
    for c in range(8):
        for hf in range(2):
            st = wst[k % 2]
            k += 1
            kb.dma("sp", st.ap, w_v[:, c, hf * 1920:(hf + 1) * 1920], writes=[st])
            kb.op("dve", lambda e, st=st, c=c, hf=hf: e.tensor_scalar(
                out=gW.ap[:, c, hf * 1920:(hf + 1) * 1920], in0=st.ap, scalar1=gm.ap[:, c:c + 1],
                scalar2=None, op0=ALU.mult), reads=[st, gm], writes=[gW])

    NB = 2
    xs = [kb.sb([128, 8, 128], F32, f"xs{i}") for i in range(NB)]
    xb = [kb.sb([128, 8, 128], BF16, f"xb{i}") for i in range(NB)]
    sq = [kb.sb([128, 8, 128], BF16, f"sq{i}") for i in range(NB)]
    cs = [kb.sb([128, 32], F32, f"cs{i}") for i in range(NB)]
    sn = [kb.sb([128, 32], F32, f"sn{i}") for i in range(NB)]
    rstd = [kb.sb([128, 1], F32, f"rstd{i}") for i in range(NB)]
    ot = [kb.sb([128, 640], F32, f"ot{i}") for i in range(NB)]
    ob = [kb.sb([128, D_IN], BF16, f"ob{i}") for i in range(NB)]
    rs8 = [kb.sb([128, 1], F32, f"rs8{i}") for i in range(NB)]
    tA = [kb.sb([128, 640], F32, f"tA{i}") for i in range(NB)]
    tB = [kb.sb([128, 640], F32, f"tB{i}") for i in range(NB)]
    st10 = [kb.sb([128, 10], F32, f"st10{i}") for i in range(NB)]
    pss = kb.ps([128, 1], F32, "pss")
    psp = [kb.ps([128, 512], F32, f"psp{i}") for i in range(4)]
    xT_v = xT.rearrange("(c p) t -> p c t", p=128)
    colchunks = [(i * 512, min(512, D_IN - i * 512)) for i in range(8)]
    pk = 0
    CATS = [(0, 640, "a"), (640, 768, "p"), (768, 1536, "q"), (1536, 3072, "p"), (3072, 3328, "q"), (3328, 3840, "p")]
    for t in range(NT):
        b = t % NB
        X, XB, SQ, CS, SN, RS, OT, TA, TB, S10 = xs[b], xb[b], sq[b], cs[b], sn[b], rstd[b], ot[b], tA[b], tB[b], st10[b]
        OB, RS8 = ob[b], rs8[b]
        kb.dma("sp", X.ap, xT_v[:, :, t * 128:(t + 1) * 128], writes=[X])
        kb.dma("sp", CS.ap, cosd[t * 128:(t + 1) * 128, :], writes=[CS])
        kb.dma("sp", SN.ap, sind[t * 128:(t + 1) * 128, :], writes=[SN])
        kb.op("dve", lambda e: e.tensor_copy(out=XB.ap, in_=X.ap), reads=[X], writes=[XB])
        kb.op("act", lambda e: e.activation(out=SQ.ap, in_=X.ap, func=AF.Square), reads=[X], writes=[SQ])
        for c in range(8):
            kb.op("pe", lambda e, c=c: e.matmul(pss.ap, lhsT=SQ.ap[:, c, :], rhs=ones.ap, start=(c == 0), stop=(c == 7)),
                  reads=[SQ, ones], writes=[pss])
        kb.op("act", lambda e: e.activation(out=RS.ap, in_=pss.ap, func=AF.Sqrt, scale=1.0 / D, bias=epsb.ap[:, 0:1]),
              reads=[pss, epsb], writes=[RS])
        kb.op("dve", lambda e: e.reciprocal(out=RS.ap, in_=RS.ap), reads=[RS], writes=[RS])
        kb.op("dve", lambda e: e.tensor_scalar(out=RS8.ap, in0=RS.ap, scalar1=0.125, scalar2=None, op0=ALU.mult), reads=[RS], writes=[RS8])
        for (c0, w) in colchunks:
            P = psp[pk % 4]
            pk += 1
            for c in range(8):
                kb.op("pe", lambda e, c=c, P=P, c0=c0, w=w: e.matmul(P.ap[:, 0:w], lhsT=XB.ap[:, c, :], rhs=gW.ap[:, c, c0:c0 + w],
                                                               start=(c == 0), stop=(c == 7)), reads=[XB, gW], writes=[P])
            for (a0, a1, cat) in CATS:
                lo, hi = max(a0, c0), min(a1, c0 + w)
                if lo >= hi:
                    continue
                if cat == "a":
                    kb.op("act", lambda e, P=P, lo=lo, hi=hi, c0=c0: e.activation(out=OT.ap[:, lo:hi], in_=P.ap[:, lo - c0:hi - c0], func=AF.Copy, scale=RS.ap[:, 0:1]),
                          reads=[P, RS], writes=[OT])
                else:
                    SC = RS8 if cat == "q" else RS
                    kb.op("act", lambda e, P=P, lo=lo, hi=hi, c0=c0, SC=SC: e.activation(out=OB.ap[:, lo:hi], in_=P.ap[:, lo - c0:hi - c0], func=AF.Copy, scale=SC.ap[:, 0:1]),
                          reads=[P, SC], writes=[OB])
        y3 = OT.ap.rearrange("p (h d) -> p h d", d=64)
        ta3 = TA.ap.rearrange("p (h d) -> p h d", d=64)
        tb3 = TB.ap.rearrange("p (h d) -> p h d", d=64)
        kb.op("dve", lambda e: e.tensor_tensor(out=TA.ap, in0=OT.ap, in1=OT.ap, op=ALU.mult), reads=[OT], writes=[TA])
        kb.op("dve", lambda e: e.tensor_reduce(out=S10.ap, in_=ta3, axis=AX.X, op=ALU.add), reads=[TA], writes=[S10])
        kb.op("act", lambda e: e.activation(out=S10.ap, in_=S10.ap, func=AF.Sqrt, scale=1.0 / HD, bias=epsb.ap[:, 0:1]), reads=[S10, epsb], writes=[S10])
        kb.op("dve", lambda e: e.reciprocal(out=S10.ap, in_=S10.ap), reads=[S10], writes=[S10])
        kb.op("dve", lambda e: e.tensor_scalar(out=S10.ap[:, 0:8], in0=S10.ap[:, 0:8], scalar1=0.125, scalar2=None, op0=ALU.mult), reads=[S10], writes=[S10])
        kb.op("dve", lambda e: e.tensor_tensor(out=ta3, in0=y3, in1=S10.ap.unsqueeze(2).to_broadcast([128, 10, 64]), op=ALU.mult), reads=[OT, S10], writes=[TA])
        kb.op("dve", lambda e: e.tensor_tensor(out=TA.ap, in0=TA.ap, in1=gq.ap, op=ALU.mult), reads=[TA, gq], writes=[TA])
        x4 = TA.ap.rearrange("p (h i two) -> p h i two", i=32, two=2)
        o4 = OB.ap[:, 0:640].rearrange("p (h i two) -> p h i two", i=32, two=2)
        t4 = TB.ap.rearrange("p (h i two) -> p h i two", i=32, two=2)
        cb = CS.ap.unsqueeze(1).to_broadcast([128, 10, 32])
        sb_ = SN.ap.unsqueeze(1).to_broadcast([128, 10, 32])
        x1, x2 = x4[:, :, :, 0], x4[:, :, :, 1]
        kb.op("dve", lambda e: e.tensor_tensor(out=t4[:, :, :, 0], in0=x1, in1=cb, op=ALU.mult), reads=[TA, CS], writes=[TB])
        kb.op("dve", lambda e: e.tensor_tensor(out=t4[:, :, :, 1], in0=x2, in1=sb_, op=ALU.mult), reads=[TA, SN], writes=[TB])
        kb.op("dve", lambda e: e.tensor_tensor(out=o4[:, :, :, 0], in0=t4[:, :, :, 0], in1=t4[:, :, :, 1], op=ALU.subtract), reads=[TB], writes=[OB])
        kb.op("dve", lambda e: e.tensor_tensor(out=t4[:, :, :, 0], in0=x1, in1=sb_, op=ALU.mult), reads=[TA, SN], writes=[TB])
        kb.op("dve", lambda e: e.tensor_tensor(out=t4[:, :, :, 1], in0=x2, in1=cb, op=ALU.mult), reads=[TA, CS], writes=[TB])
        kb.op("dve", lambda e: e.tensor_tensor(out=o4[:, :, :, 1], in0=t4[:, :, :, 0], in1=t4[:, :, :, 1], op=ALU.add), reads=[TB], writes=[OB])
        kb.dma("pool", proj[t * 128:(t + 1) * 128, :], OB.ap, reads=[OB], is_output=True)
    return kb


def rope_tables(pos):
    row = (pos // 64).astype(np.float32)
    col = (pos % 64).astype(np.float32)
    half = HD // 2
    freqs = (np.float32(10000.0) ** (-np.arange(0, half, 2, dtype=np.float32) / np.float32(half))).astype(np.float32)
    ang = np.concatenate([row[:, None] * freqs, col[:, None] * freqs], axis=-1).astype(np.float32)
    return np.cos(ang).astype(np.float32), np.sin(ang).astype(np.float32)


def core_tokens(xp, xs, c):
    return np.concatenate([xp[c * SEG:(c + 1) * SEG]] + [xs[4 * c + i] for i in range(4)], axis=0)


def run_l1(xcores, w_in_l, norm_mix_l, q_gain_l, k_gain_l):
    kb = build_l1()
    gq = np.concatenate([np.tile(q_gain_l, 8), np.tile(k_gain_l, 2)]).astype(np.float32)
    maps = []
    for c in range(NCORES):
        pos = np.concatenate([np.arange(c * SEG, (c + 1) * SEG)] + [np.arange(SEG)] * 4)
        cs, sn = rope_tables(pos)
        maps.append({
            "xT": np.ascontiguousarray(xcores[c].T),
            "w_in": np.ascontiguousarray(w_in_l),
            "gmix": np.ascontiguousarray(norm_mix_l.reshape(8, 128).T),
            "gqk": np.ascontiguousarray(np.broadcast_to(gq, (128, 640))),
            "cos": cs, "sin": sn,
        })
    res = _run(kb, maps)
    return [r["proj"] for r in res]


PADSEG = 4096
HALO = 1024
B_GR = [(1, 64), (4, 256), (16, 1024)]
B_NT = [5, 8, 20]
B_SW = [128 * (n - 1) + 512 for n in B_NT]
B_SO = [0, B_SW[0], B_SW[0] + B_SW[1]]
B_STRIP = sum(B_SW)
C_NT = 8
NQB = NTOK // 512


def build_l2():
    kb = KB()
    QT = kb.dram("QT", [24, 64, NTOK], BF16, "ExternalInput")
    KTAp = kb.dram("KTAp", [2, 64, SEQ_P], BF16, "ExternalInput")
    VAp = kb.dram("VAp", [2, SEQ_P, 65], BF16, "ExternalInput")
    KTAs = kb.dram("KTAs", [2, 64, 4 * SEG], BF16, "ExternalInput")
    VAs = kb.dram("VAs", [2, 4 * SEG, 65], BF16, "ExternalInput")
    KTB = kb.dram("KTB", [12, 64, NSEG * PADSEG], BF16, "ExternalInput")
    VB = kb.dram("VB", [12, NSEG * PADSEG, 65], BF16, "ExternalInput")
    KTC = kb.dram("KTC", [4, 64, NSEG * PADSEG], BF16, "ExternalInput")
    VC = kb.dram("VC", [4, NSEG * PADSEG, 65], BF16, "ExternalInput")
    stripB = kb.dram("stripB", [128, 4, B_STRIP], F32, "ExternalInput")
    biasC = kb.dram("biasC", [5, 4, C_NT, 128, 512], F32, "ExternalInput")
    xin = kb.dram("x", [NTOK, D], F32, "ExternalInput")
    w_out = kb.dram("w_out", [D, D], F32, "ExternalInput")
    gout = kb.dram("gout", [64, 16], F32, "ExternalInput")
    g2d = kb.dram("g2", [128, 8], F32, "ExternalInput")
    w_r = kb.dram("w_r", [D, NEXP], F32, "ExternalInput")
    xmid = kb.dram("xmid", [NTOK, D], F32, "ExternalOutput")
    affo = kb.dram("aff", [NTOK, NEXP], F32, "ExternalOutput")

    stage = kb.sb([128, 4096], F32, "stage")
    sB = kb.sb([128, 4, B_STRIP], BF16, "sB")
    gWo = kb.sb([64, 16, D], BF16, "gWo")
    go = kb.sb([64, 16], F32, "go")
    g2 = kb.sb([128, 8], F32, "g2sb")
    identb = kb.sb([128, 128], BF16, "identb")
    identf = kb.sb([128, 128], F32, "identf")
    onesb = kb.sb([128, 64], BF16, "onesb")
    epsb = kb.sb([128, 1], F32, "epsb")
    gw32 = kb.sb([128, 8, NEXP], F32, "gw32")
    gwh = kb.sb([128, 8, NEXP], BF16, "gwh")
    gwl = kb.sb([128, 8, NEXP], BF16, "gwl")
    kb.op("pool", lambda e: e.memset(identf.ap, 0.0), writes=[identf])
    kb.op("pool", lambda e: e.affine_select(out=identf.ap, in_=identf.ap, pattern=[[-1, 128]], compare_op=ALU.not_equal,
                                            fill=1.0, base=0, channel_multiplier=1), reads=[identf], writes=[identf])
    kb.op("dve", lambda e: e.tensor_copy(out=identb.ap, in_=identf.ap), reads=[identf], writes=[identb])
    kb.op("pool", lambda e: e.memset(onesb.ap, 1.0), writes=[onesb])
    kb.op("pool", lambda e: e.memset(epsb.ap, EPS), writes=[epsb])
    kb.dma("sp", go.ap, gout, writes=[go])
    kb.dma("sp", g2.ap, g2d, writes=[g2])
    for h in range(4):
        for (o, w) in ((0, 4096), (4096, B_STRIP - 4096)):
            kb.dma("sp", stage.ap[:, 0:w], stripB[:, h, o:o + w], writes=[stage])
            kb.op("dve", lambda e, h=h, o=o, w=w: e.tensor_copy(out=sB.ap[:, h, o:o + w], in_=stage.ap[:, 0:w]), reads=[stage], writes=[sB])
    wo_v = w_out.rearrange("(s d) n -> d s n", d=64)
    for s4 in range(4):
        stv = stage.ap[0:64, :].rearrange("p (s n) -> p s n", s=4)
        kb.dma("sp", stv, wo_v[:, s4 * 4:(s4 + 1) * 4, :], writes=[stage])
        for s in range(4):
            sl = s4 * 4 + s
            kb.op("dve", lambda e, s=s, sl=sl, stv=stv: e.tensor_scalar(out=gWo.ap[:, sl, :], in0=stv[:, s, :], scalar1=go.ap[:, sl:sl + 1],
                                                                        scalar2=None, op0=ALU.mult), reads=[stage, go], writes=[gWo])
    kb.dma("sp", gw32.ap, w_r.rearrange("(c p) e -> p c e", p=128), writes=[gw32])
    kb.op("dve", lambda e: e.tensor_tensor(out=gw32.ap, in0=gw32.ap, in1=g2.ap.unsqueeze(2).to_broadcast([128, 8, NEXP]), op=ALU.mult),
          reads=[gw32, g2], writes=[gw32])
    kb.op("dve", lambda e: e.tensor_copy(out=gwh.ap, in_=gw32.ap), reads=[gw32], writes=[gwh])
    kb.op("dve", lambda e: e.tensor_tensor(out=gwl.ap, in0=gw32.ap, in1=gwh.ap, op=ALU.subtract), reads=[gw32, gwh], writes=[gwl])

    qbuf = [kb.sb([64, 4, 512], BF16, f"qbuf{i}") for i in range(2)]
    kbuf = [kb.sb([64, 2560], BF16, f"kbuf{i}") for i in range(2)]
    vbuf = [kb.sb([128, 20, 65], BF16, f"vbuf{i}") for i in range(2)]
    bcb = [kb.sb([128, C_NT, 512], BF16, f"bcb{i}") for i in range(2)]
    PT = [kb.sb([128, 512], BF16, f"PT{i}") for i in range(3)]
    mixT = kb.sb([64, 16, 512], BF16, "mixT")
    osq = kb.sb([64, 16, 128], BF16, "osq")
    rden = kb.sb([128, 512], F32, "rden")
    rh = kb.sb([128, 512], BF16, "rh")
    rl = kb.sb([128, 512], BF16, "rl")
    bcs = kb.sb([64, 512], F32, "bcs")
    xm = [kb.sb([128, D], F32, f"xm{i}") for i in range(2)]
    xh = kb.sb([128, 8, 128], BF16, "xh")
    xl = kb.sb([128, 8, 128], BF16, "xl")
    junk = kb.sb([128, D], BF16, "junk")
    sm = [kb.sb([128, 8], F32, f"sm{i}") for i in range(2)]
    lg = [kb.sb([128, NEXP], F32, f"lg{i}") for i in range(2)]
    acc = [kb.ps([128, 512], F32, f"acc{i}") for i in range(4)]
    stp = [kb.ps([128, 512], F32, f"stp{i}") for i in range(2)]
    bcp = kb.ps([128, 512], F32, "bcp")
    cnt = {"k": 0, "q": 0, "s": 0, "p": 0, "c": 0}

    def nxt(name, lst):
        b = lst[cnt[name] % len(lst)]
        cnt[name] += 1
        return b

    def score_pv(K, koff, Q, qs, V, vt, A, first, last, bias=None):
        S = nxt("s", stp)
        P = nxt("p", PT)
        kb.op("pe", lambda e: e.matmul(S.ap, lhsT=K.ap[:, koff:koff + 128], rhs=Q.ap[:, qs, :], start=True, stop=(bias is None)),
              reads=[K, Q], writes=[S])
        if bias is not None:
            bbuf, bap = bias
            kb.op("pe", lambda e: e.matmul(S.ap, lhsT=identb.ap, rhs=bap, start=False, stop=True), reads=[identb, bbuf], writes=[S])
        kb.op("act", lambda e: e.activation(out=P.ap, in_=S.ap, func=AF.Exp), reads=[S], writes=[P])
        kb.op("pe", lambda e: e.matmul(A.ap[0:65, :], lhsT=V.ap[:, vt, :], rhs=P.ap, start=first, stop=last), reads=[V, P], writes=[A])

    def finalize(A, slot):
        kb.op("dve", lambda e: e.reciprocal(out=rden.ap[64:65, :], in_=A.ap[64:65, :]), reads=[A], writes=[rden])
        kb.op("dve", lambda e: e.tensor_copy(out=rh.ap[64:65, :], in_=rden.ap[64:65, :]), reads=[rden], writes=[rh])
        kb.op("dve", lambda e: e.tensor_tensor(out=rl.ap[64:65, :], in0=rden.ap[64:65, :], in1=rh.ap[64:65, :], op=ALU.subtract),
              reads=[rden, rh], writes=[rl])
        kb.op("pe", lambda e: e.matmul(bcp.ap[0:64, :], lhsT=onesb.ap[64:65, 0:64], rhs=rh.ap[64:65, :], start=True, stop=False),
              reads=[onesb, rh], writes=[bcp])
        kb.op("pe", lambda e: e.matmul(bcp.ap[0:64, :], lhsT=onesb.ap[64:65, 0:64], rhs=rl.ap[64:65, :], start=False, stop=True),
              reads=[onesb, rl], writes=[bcp])
        kb.op("act", lambda e: e.activation(out=bcs.ap, in_=bcp.ap[0:64, :], func=AF.Copy), reads=[bcp], writes=[bcs])
        kb.op("dve", lambda e: e.tensor_tensor(out=mixT.ap[:, slot, :], in0=A.ap[0:64, :], in1=bcs.ap, op=ALU.mult), reads=[A, bcs], writes=[mixT])

    def load_kv(KTd, Vd, col0, ntile):
        K = nxt("k", kbuf)
        V = vbuf[(cnt["k"] - 1) % 2]
        kb.dma("sp", K.ap[:, 0:ntile * 128], KTd[:, col0:col0 + ntile * 128], writes=[K])
        kb.dma("sp", V.ap[:, 0:ntile, :], Vd[col0:col0 + ntile * 128, :].rearrange("(t p) c -> p t c", p=128), writes=[V])
        return K, V

    for qb in range(NQB):
        seg, qbl = qb // 4, qb % 4
        t0 = qb * 512
        for j in range(2):
            Q = nxt("q", qbuf)
            kb.dma("sp", Q.ap, QT[4 * j:4 * j + 4, :, t0:t0 + 512].rearrange("s d t -> d s t"), writes=[Q])
            if seg == 0:
                chunks = [(KTAp[j], VAp[j], kc * 2048) for kc in range(8)]
            else:
                chunks = [(KTAs[j], VAs[j], (seg - 1) * SEG)]
            for ci, (KTd, Vd, col0) in enumerate(chunks):
                K, V = load_kv(KTd, Vd, col0, 16)
                for kt in range(16):
                    first = (ci == 0 and kt == 0)
                    last = (ci == len(chunks) - 1 and kt == 15)
                    for qh in range(4):
                        score_pv(K, kt * 128, Q, qh, V, kt, acc[qh], first, last)
            for qh in range(4):
                finalize(acc[qh], 4 * j + qh)
        for h in range(4):
            Q = nxt("q", qbuf)
            kb.dma("sp", Q.ap[:, 0:3, :], QT[8 + h:20:4, :, t0:t0 + 512].rearrange("s d t -> d s t"), writes=[Q])
            A = acc[h]
            for g in range(3):
                reach, nt = B_GR[g][1], B_NT[g]
                col0 = seg * PADSEG + HALO + 512 * qbl - reach
                K, V = load_kv(KTB[g * 4 + h], VB[g * 4 + h], col0, nt)
                for kt in range(nt):
                    so = B_SO[g] + 128 * (nt - 1 - kt)
                    score_pv(K, kt * 128, Q, g, V, kt, A, (g == 0 and kt == 0), (g == 2 and kt == nt - 1),
                             bias=(sB, sB.ap[:, h, so:so + 512]))
            finalize(A, 8 + h)
        if seg == 0:
            cls = {0: 3, 3: 4}.get(qbl, 1)
        else:
            cls = {0: 0, 3: 2}.get(qbl, 1)
        for h in range(4):
            Q = nxt("q", qbuf)
            kb.dma("sp", Q.ap[:, 0:1, :], QT[20 + h:21 + h, :, t0:t0 + 512].rearrange("s d t -> d s t"), writes=[Q])
            A = acc[h]
            col0 = seg * PADSEG + HALO + 512 * qbl - 256
            K, V = load_kv(KTC[h], VC[h], col0, C_NT)
            BC = nxt("c", bcb)
            stv = stage.ap.rearrange("p (k n) -> p k n", k=C_NT)
            kb.dma("sp", stv, biasC[cls, h].rearrange("k p n -> p k n"), writes=[stage])
            kb.op("dve", lambda e, BC=BC, stv=stv: e.tensor_copy(out=BC.ap, in_=stv), reads=[stage], writes=[BC])
            for kt in range(C_NT):
                score_pv(K, kt * 128, Q, 0, V, kt, A, kt == 0, kt == C_NT - 1, bias=(BC, BC.ap[:, kt, :]))
            finalize(A, 12 + h)
        for tt in range(4):
            tok0 = t0 + tt * 128
            X = xm[tt % 2]
            SM = sm[tt % 2]
            LG = lg[tt % 2]
            kb.dma("sp", X.ap, xin[tok0:tok0 + 128, :], writes=[X])
            kb.op("dve", lambda e: e.tensor_tensor(out=osq.ap, in0=mixT.ap[:, :, tt * 128:(tt + 1) * 128], in1=mixT.ap[:, :, tt * 128:(tt + 1) * 128], op=ALU.mult),
                  reads=[mixT], writes=[osq])
            for m, (s0, s1, width) in enumerate(((0, 8, 512), (8, 12, 256), (12, 16, 256))):
                for s in range(s0, s1):
                    kb.op("pe", lambda e, s=s: e.matmul(acc[2].ap[:, 0:1], lhsT=osq.ap[:, s, :], rhs=onesb.ap[0:64, 0:1], start=(s == s0), stop=(s == s1 - 1)),
                          reads=[osq, onesb], writes=[acc[2]])
                kb.op("act", lambda e, m=m, width=width: e.activation(out=SM.ap[:, m:m + 1], in_=acc[2].ap[:, 0:1], func=AF.Sqrt, scale=1.0 / width, bias=epsb.ap[:, 0:1]),
                      reads=[acc[2], epsb], writes=[SM])
                kb.op("dve", lambda e, m=m: e.reciprocal(out=SM.ap[:, m:m + 1], in_=SM.ap[:, m:m + 1]), reads=[SM], writes=[SM])
                for half in range(2):
                    O = acc[half]
                    for s in range(s0, s1):
                        kb.op("pe", lambda e, s=s, O=O, half=half: e.matmul(O.ap, lhsT=mixT.ap[:, s, tt * 128:(tt + 1) * 128], rhs=gWo.ap[:, s, half * 512:(half + 1) * 512],
                                                                       start=(s == s0), stop=(s == s1 - 1)), reads=[mixT, gWo], writes=[O])
                    kb.op("dve", lambda e, O=O, half=half, m=m: e.scalar_tensor_tensor(out=X.ap[:, half * 512:(half + 1) * 512], in0=O.ap, scalar=SM.ap[:, m:m + 1],
                                                                                     in1=X.ap[:, half * 512:(half + 1) * 512], op0=ALU.mult, op1=ALU.add),
                          reads=[O, SM, X], writes=[X])
            kb.dma("pool", xmid[tok0:tok0 + 128, :], X.ap, reads=[X], is_output=True)
            kb.op("act", lambda e: e.activation(out=junk.ap, in_=X.ap, func=AF.Square, accum_out=SM.ap[:, 3:4]), reads=[X], writes=[junk, SM])
            kb.op("act", lambda e: e.activation(out=SM.ap[:, 3:4], in_=SM.ap[:, 3:4], func=AF.Sqrt, scale=1.0 / D, bias=epsb.ap[:, 0:1]), reads=[SM, epsb], writes=[SM])
            kb.op("dve", lambda e: e.reciprocal(out=SM.ap[:, 3:4], in_=SM.ap[:, 3:4]), reads=[SM], writes=[SM])
            for c in range(8):
                kb.op("pe", lambda e, c=c: e.transpose(acc[3].ap[:, (c % 4) * 128:(c % 4 + 1) * 128], X.ap[:, c * 128:(c + 1) * 128], identf.ap),
                      reads=[X, identf], writes=[acc[3]])
                kb.op("act", lambda e, c=c: e.activation(out=xh.ap[:, c, :], in_=acc[3].ap[:, (c % 4) * 128:(c % 4 + 1) * 128], func=AF.Copy), reads=[acc[3]], writes=[xh])
                kb.op("dve", lambda e, c=c: e.tensor_tensor(out=xl.ap[:, c, :], in0=acc[3].ap[:, (c % 4) * 128:(c % 4 + 1) * 128], in1=xh.ap[:, c, :], op=ALU.subtract),
                      reads=[acc[3], xh], writes=[xl])
            k = 0
            for c in range(8):
                for (xa, wa) in ((xh, gwh), (xh, gwl), (xl, gwh)):
                    kb.op("pe", lambda e, c=c, xa=xa, wa=wa, k=k: e.matmul(acc[2].ap[:, 16:32], lhsT=xa.ap[:, c, :], rhs=wa.ap[:, c, :], start=(k == 0), stop=(k == 23)),
                          reads=[xa, wa], writes=[acc[2]])
                    k += 1
            kb.op("act", lambda e: e.activation(out=LG.ap, in_=acc[2].ap[:, 16:32], func=AF.Copy, scale=SM.ap[:, 3:4]), reads=[acc[2], SM], writes=[LG])
            kb.op("dve", lambda e: e.tensor_reduce(out=SM.ap[:, 4:5], in_=LG.ap, axis=AX.X, op=ALU.max), reads=[LG], writes=[SM])
            kb.op("dve", lambda e: e.tensor_scalar(out=SM.ap[:, 4:5], in0=SM.ap[:, 4:5], scalar1=-1.0, scalar2=None, op0=ALU.mult), reads=[SM], writes=[SM])
            kb.op("act", lambda e: e.activation(out=LG.ap, in_=LG.ap, func=AF.Exp, bias=SM.ap[:, 4:5], accum_out=SM.ap[:, 5:6]), reads=[LG, SM], writes=[LG, SM])
            kb.op("dve", lambda e: e.reciprocal(out=SM.ap[:, 5:6], in_=SM.ap[:, 5:6]), reads=[SM], writes=[SM])
            kb.op("dve", lambda e: e.tensor_scalar(out=LG.ap, in0=LG.ap, scalar1=SM.ap[:, 5:6], scalar2=None, op0=ALU.mult), reads=[LG, SM], writes=[LG])
            kb.dma("pool", affo[tok0:tok0 + 128, :], LG.ap, reads=[LG], is_output=True)
    return kb


def _t5_bucket(rel):
    nb, max_exact = 16, 8
    ret = (rel > 0).astype(np.int32) * nb
    n = np.abs(rel)
    lg = np.log(np.maximum(n, 1).astype(np.float32) / np.float32(max_exact)) / np.float32(np.log(2048.0 / max_exact)) * np.float32(nb - max_exact)
    large = np.minimum(max_exact + lg.astype(np.float32).astype(np.int32), nb - 1)
    return ret + np.where(n < max_exact, n, large)


def _strip_b(rel_bias):
    out = np.full((128, 4, B_STRIP), NEG, np.float32)
    i = np.arange(128)[:, None]
    for g, (d, reach) in enumerate(B_GR):
        nt, w = B_NT[g], B_SW[g]
        m = np.arange(w)[None, :]
        rel = i - m + 128 * (nt - 1) - reach
        valid = (rel % d == 0) & (np.abs(rel) <= 64 * d)
        bk = _t5_bucket(rel)
        for h in range(4):
            vals = rel_bias[bk, g * 4 + h]
            out[:, h, B_SO[g]:B_SO[g] + w] = np.where(valid, vals, np.float32(NEG))
    return out


def _bias_c(rpb, R, rows):
    out = np.full((4, C_NT, 128, 512), NEG, np.float32)
    r = (8 * R + np.arange(8))[:, None].repeat(64, 1).reshape(-1)
    c = np.tile(np.arange(64), 8)
    rs = np.clip(r - 4, 0, rows - 8)
    cs = np.clip(c - 8, 0, 64 - 16)
    for kt in range(C_NT):
        kr = (8 * R - 4 + 2 * kt + np.arange(2))[:, None].repeat(64, 1).reshape(-1)
        ck = np.tile(np.arange(64), 2)
        valid = ((kr[:, None] >= rs[None, :]) & (kr[:, None] <= rs[None, :] + 7) & (kr[:, None] >= 0) & (kr[:, None] < rows)
                 & (ck[:, None] >= cs[None, :]) & (ck[:, None] < cs[None, :] + 16))
        dr = np.clip(kr[:, None] - r[None, :] + 7, 0, 14)
        dc = np.clip(ck[:, None] - c[None, :], -15, 15) + 15
        for h in range(4):
            out[h, kt] = np.where(valid, rpb[h][dr, dc], np.float32(NEG))
    return out


def _aug(v, valid):
    return np.concatenate([v, valid[:, None].astype(v.dtype)], axis=1)


def run_l2(projs, xcores, w_out_l, out_gain_l, norm_ffn_l, w_r_l, rel_bias, na_rpb_l):
    kb = build_l2()
    Pp = np.concatenate([projs[c][:SEG] for c in range(NCORES)], 0)
    one_p = np.ones(SEQ_P, bool)
    KTAp = np.ascontiguousarray(np.stack([Pp[:, 512 + 64 * j:576 + 64 * j].T for j in range(2)]))
    VAp = np.ascontiguousarray(np.stack([_aug(Pp[:, 640 + 64 * j:704 + 64 * j], one_p) for j in range(2)]))
    stripB = _strip_b(rel_bias)
    bc_first, bc_mid, bc_last = _bias_c(na_rpb_l, 0, 32), _bias_c(na_rpb_l, 1, 32), _bias_c(na_rpb_l, 3, 32)
    qcols = [64 * s for s in range(8)] + [768 + 64 * k for k in range(12)] + [3072 + 64 * h for h in range(4)]
    maps = []
    for c in range(NCORES):
        loc = projs[c]
        QT = np.ascontiguousarray(np.stack([loc[:, q:q + 64].T for q in qcols]))
        ls = loc[SEG:]
        one_s = np.ones(4 * SEG, bool)
        KTAs = np.ascontiguousarray(np.stack([ls[:, 512 + 64 * j:576 + 64 * j].T for j in range(2)]))
        VAs = np.ascontiguousarray(np.stack([_aug(ls[:, 640 + 64 * j:704 + 64 * j], one_s) for j in range(2)]))
        pad = np.zeros((NSEG * PADSEG, D_IN), loc.dtype)
        val = np.zeros(NSEG * PADSEG, bool)
        g0, g1 = max(0, SEG * c - HALO), min(SEQ_P, SEG * c + SEG + HALO)
        o0 = g0 - (SEG * c - HALO)
        pad[o0:o0 + (g1 - g0)] = Pp[g0:g1]
        val[o0:o0 + (g1 - g0)] = True
        for s in range(1, NSEG):
            pad[s * PADSEG + HALO:s * PADSEG + HALO + SEG] = loc[s * SEG:(s + 1) * SEG]
            val[s * PADSEG + HALO:s * PADSEG + HALO + SEG] = True
        KTB = np.ascontiguousarray(np.stack([pad[:, 1536 + 64 * k:1600 + 64 * k].T for k in range(12)]))
        VB = np.ascontiguousarray(np.stack([_aug(pad[:, 2304 + 64 * k:2368 + 64 * k], val) for k in range(12)]))
        KTC = np.ascontiguousarray(np.stack([pad[:, 3328 + 64 * h:3392 + 64 * h].T for h in range(4)]))
        VC = np.ascontiguousarray(np.stack([_aug(pad[:, 3584 + 64 * h:3648 + 64 * h], val) for h in range(4)]))
        biasC = np.stack([bc_first, bc_mid, bc_last, _bias_c(na_rpb_l, 4 * c, 256), _bias_c(na_rpb_l, 4 * c + 3, 256)])
        maps.append({
            "QT": QT, "KTAp": KTAp, "VAp": VAp, "KTAs": KTAs, "VAs": VAs, "KTB": KTB, "VB": VB, "KTC": KTC, "VC": VC,
            "stripB": stripB, "biasC": np.ascontiguousarray(biasC), "x": np.ascontiguousarray(xcores[c]),
            "w_out": np.ascontiguousarray(w_out_l), "gout": np.ascontiguousarray(out_gain_l.reshape(16, 64).T),
            "g2": np.ascontiguousarray(norm_ffn_l.reshape(8, 128).T), "w_r": np.ascontiguousarray(w_r_l),
        })
    res = _run(kb, maps)
    return [r["xmid"] for r in res], [r["aff"] for r in res]


TG = 1024
NG = NTOK // TG
JB = 256
NJB = DEXP // JB
NBIS = 34


def build_l3(final):
    kb = KB()
    xmT = kb.dram("xmT", [D, NTOK], F32, "ExternalInput")
    xmid = kb.dram("xmid", [NTOK, D], F32, "ExternalInput")
    affl = kb.dram("affl", [NTOK, NEXP], F32, "ExternalInput")
    affP = kb.dram("affP", [128, SEQ_P // 8], F32, "ExternalInput")
    affS = kb.dram("affS", [128, NSEQ_S * SEQ_S // 8], F32, "ExternalInput")
    BDd = kb.dram("BD", [128, 128], F32, "ExternalInput")
    Seld = kb.dram("Sel", [128, NEXP], F32, "ExternalInput")
    g2d = kb.dram("g2", [128, 8], F32, "ExternalInput")
    gfd = kb.dram("gfin", [128, D], F32, "ExternalInput")
    wg = kb.dram("wg", [NEXP, D, DEXP], F32, "ExternalInput")
    wu = kb.dram("wu", [NEXP, D, DEXP], F32, "ExternalInput")
    wd = kb.dram("wd", [NEXP, DEXP, D], F32, "ExternalInput")
    xo = kb.dram("xo", [NTOK, D], F32, "ExternalOutput")

    big = kb.sb([128, 12288], F32, "big")
    AS = big.ap[:, 0:8192]
    MK = big.ap[:, 8192:12288].bitcast(BF16)
    APb = kb.sb([128, 2048], F32, "APb")
    BD32 = kb.sb([128, 128], F32, "BD32")
    BD = kb.sb([128, 128], BF16, "BDb")
    Sel = kb.sb([128, NEXP], F32, "Selb")
    g2 = kb.sb([128, 8], F32, "g2sb")
    gf = kb.sb([128, D], F32, "gfsb")
    onesb = kb.sb([128, 128], BF16, "onesb")
    epsb = kb.sb([128, 1], F32, "epsb")
    AL = kb.sb([128, NT, NEXP], F32, "AL")
    GT = kb.sb([128, NT, NEXP], F32, "GT")
    small = {n: kb.sb([128, 1], F32, "sm_" + n) for n in ("lo", "hi", "md", "c1", "cl32", "ge", "nge", "t1", "t2", "rem", "pf")}
    chb = kb.sb([128, 1], BF16, "chb")
    clb = kb.sb([128, 1], BF16, "clb")
    pb = kb.sb([128, 1], BF16, "pb")
    R32 = kb.sb([128, NEXP], F32, "R32")
    Rb = [kb.sb([128, NEXP], BF16, f"Rb{i}") for i in range(3)]
    Tthr = [kb.sb([128, NEXP], F32, f"Tthr{i}") for i in range(2)]
    psm = kb.ps([128, 512], F32, "psm")
    kb.op("pool", lambda e: e.memset(onesb.ap, 1.0), writes=[onesb])
    kb.op("pool", lambda e: e.memset(epsb.ap, EPS), writes=[epsb])
    kb.dma("sp", BD32.ap, BDd, writes=[BD32])
    kb.op("dve", lambda e: e.tensor_copy(out=BD.ap, in_=BD32.ap), reads=[BD32], writes=[BD])
    kb.dma("sp", Sel.ap, Seld, writes=[Sel])
    kb.dma("sp", g2.ap, g2d, writes=[g2])
    kb.dma("sp", gf.ap, gfd, writes=[gf])
    kb.dma("sp", AL.ap, affl.rearrange("(t p) e -> p t e", p=128), writes=[AL])
    kb.dma("sp", AS, affS, writes=[big])
    kb.dma("sp", APb.ap, affP, writes=[APb])
    S = small

    def dve(fn, reads, writes):
        return kb.op("dve", fn, reads=reads, writes=writes)

    for gi, (Abuf, Aap, n, cap) in enumerate(((APb, APb.ap, SEQ_P // 8, SEQ_P // 8), (big, AS, NSEQ_S * SEQ_S // 8, NSEQ_S * SEQ_S // 8))):
        dve(lambda e: e.memset(S["lo"].ap, 0.0), [], [S["lo"]])
        dve(lambda e: e.memset(S["hi"].ap, 1.0), [], [S["hi"]])
        for it in range(NBIS):
            dve(lambda e: e.tensor_tensor(out=S["md"].ap, in0=S["lo"].ap, in1=S["hi"].ap, op=ALU.add), [S["lo"], S["hi"]], [S["md"]])
            dve(lambda e: e.tensor_scalar(out=S["md"].ap, in0=S["md"].ap, scalar1=0.5, scalar2=None, op0=ALU.mult), [S["md"]], [S["md"]])
            dve(lambda e: e.tensor_scalar(out=MK[:, 0:n], in0=Aap, scalar1=S["md"].ap[:, 0:1], scalar2=None, op0=ALU.is_ge), [Abuf, S["md"]], [big])
            dve(lambda e: e.tensor_reduce(out=S["c1"].ap, in_=MK[:, 0:n], axis=AX.X, op=ALU.add), [big], [S["c1"]])
            dve(lambda e: e.tensor_copy(out=chb.ap, in_=S["c1"].ap), [S["c1"]], [chb])
            dve(lambda e: e.tensor_tensor(out=clb.ap, in0=S["c1"].ap, in1=chb.ap, op=ALU.subtract), [S["c1"], chb], [clb])
            kb.op("pe", lambda e: e.matmul(psm.ap[:, 0:1], lhsT=BD.ap, rhs=chb.ap, start=True, stop=False), reads=[BD, chb], writes=[psm])
            kb.op("pe", lambda e: e.matmul(psm.ap[:, 0:1], lhsT=BD.ap, rhs=clb.ap, start=False, stop=True), reads=[BD, clb], writes=[psm])
            dve(lambda e: e.tensor_scalar(out=S["ge"].ap, in0=psm.ap[:, 0:1], scalar1=float(cap), scalar2=None, op0=ALU.is_ge), [psm], [S["ge"]])
            dve(lambda e: e.tensor_scalar(out=S["nge"].ap, in0=S["ge"].ap, scalar1=-1.0, scalar2=1.0, op0=ALU.mult, op1=ALU.add), [S["ge"]], [S["nge"]])
            dve(lambda e: e.tensor_tensor(out=S["t1"].ap, in0=S["lo"].ap, in1=S["nge"].ap, op=ALU.mult), [S["lo"], S["nge"]], [S["t1"]])
            dve(lambda e: e.scalar_tensor_tensor(out=S["lo"].ap, in0=S["md"].ap, scalar=S["ge"].ap[:, 0:1], in1=S["t1"].ap, op0=ALU.mult, op1=ALU.add),
                [S["md"], S["ge"], S["t1"]], [S["lo"]])
            dve(lambda e: e.tensor_tensor(out=S["t2"].ap, in0=S["hi"].ap, in1=S["ge"].ap, op=ALU.mult), [S["hi"], S["ge"]], [S["t2"]])
            dve(lambda e: e.scalar_tensor_tensor(out=S["hi"].ap, in0=S["md"].ap, scalar=S["nge"].ap[:, 0:1], in1=S["t2"].ap, op0=ALU.mult, op1=ALU.add),
                [S["md"], S["nge"], S["t2"]], [S["hi"]])
        dve(lambda e: e.tensor_copy(out=S["rem"].ap, in_=S["lo"].ap), [S["lo"]], [S["rem"]])
        for k in range(3):
            dve(lambda e: e.tensor_copy(out=pb.ap, in_=S["rem"].ap), [S["rem"]], [pb])
            dve(lambda e: e.tensor_copy(out=S["pf"].ap, in_=pb.ap), [pb], [S["pf"]])
            dve(lambda e: e.tensor_tensor(out=S["rem"].ap, in0=S["rem"].ap, in1=S["pf"].ap, op=ALU.subtract), [S["rem"], S["pf"]], [S["rem"]])
            dve(lambda e: e.tensor_scalar(out=R32.ap, in0=Sel.ap, scalar1=S["pf"].ap[:, 0:1], scalar2=None, op0=ALU.mult), [Sel, S["pf"]], [R32])
            dve(lambda e, k=k: e.tensor_copy(out=Rb[k].ap, in_=R32.ap), [R32], [Rb[k]])
        for k in range(3):
            kb.op("pe", lambda e, k=k: e.matmul(psm.ap[:, 16:32], lhsT=onesb.ap, rhs=Rb[k].ap, start=(k == 0), stop=(k == 2)), reads=[onesb, Rb[k]], writes=[psm])
        dve(lambda e, gi=gi: e.tensor_copy(out=Tthr[gi].ap, in_=psm.ap[:, 16:32]), [psm], [Tthr[gi]])
    for (t0, t1, gi) in ((0, 16, 0), (16, NT, 1)):
        nt_ = t1 - t0
        dve(lambda e, t0=t0, t1=t1, gi=gi, nt_=nt_: e.tensor_tensor(out=GT.ap[:, t0:t1, :], in0=AL.ap[:, t0:t1, :],
                                                                in1=Tthr[gi].ap.unsqueeze(1).to_broadcast([128, nt_, NEXP]), op=ALU.is_ge), [AL, Tthr[gi]], [GT])
        dve(lambda e, t0=t0, t1=t1: e.tensor_tensor(out=GT.ap[:, t0:t1, :], in0=GT.ap[:, t0:t1, :], in1=AL.ap[:, t0:t1, :], op=ALU.mult), [GT, AL], [GT])

    stg = [kb.sb([128, 8, JB], F32, f"stg{i}") for i in range(3)]
    Wg = [kb.sb([128, 8, JB], BF16, f"Wg{i}") for i in range(2)]
    Wu = [kb.sb([128, 8, JB], BF16, f"Wu{i}") for i in range(2)]
    Wd = [kb.sb([128, JB // 128, D], BF16, f"Wd{i}") for i in range(2)]
    hT = kb.sb([128, 8, TG], BF16, "hT")
    sq = kb.sb([128, 8, JB], BF16, "sq")
    rb = kb.sb([128, JB], F32, "rb")
    hid = [kb.sb([128, JB // 128, 512], BF16, f"hid{i}") for i in range(2)]
    sg = [kb.sb([128, 512], F32, f"sg{i}") for i in range(2)]
    fin = [kb.sb([128, D], F32, f"fin{i}") for i in range(2)]
    junk = kb.sb([128, D], BF16, "junk")
    fs = [kb.sb([128, 2], F32, f"fs{i}") for i in range(2)]
    gps = [kb.ps([128, 512], F32, f"gps{i}") for i in range(2)]
    ups = [kb.ps([128, 512], F32, f"ups{i}") for i in range(2)]
    yps = [kb.ps([128, 512], F32, f"yps{i}") for i in range(2)]
    XA = big.ap[:, 0:8 * D].rearrange("p (t d) -> p t d", d=D)
    xT_v = xmT.rearrange("(c p) t -> p c t", p=128)
    wg_v = wg.rearrange("e (c p) j -> e p c j", p=128)
    wu_v = wu.rearrange("e (c p) j -> e p c j", p=128)
    wd_v = wd.rearrange("e (jc p) n -> e p jc n", p=128)
    ctr = {"s": 0, "w": 0, "g": 0, "y": 0, "h": 0}
    for G in range(NG):
        tok0 = G * TG
        kb.dma("sp", XA, xmid[tok0:tok0 + TG, :].rearrange("(t p) d -> p t d", p=128), writes=[big])
        for q in range(TG // JB):
            st = stg[ctr["s"] % 3]; ctr["s"] += 1
            kb.dma("sp", st.ap, xT_v[:, :, tok0 + q * JB:tok0 + (q + 1) * JB], writes=[st])
            kb.op("act", lambda e, st=st: e.activation(out=sq.ap, in_=st.ap, func=AF.Square), reads=[st], writes=[sq])
            for c in range(8):
                kb.op("pe", lambda e, c=c: e.matmul(psm.ap[:, 0:JB], lhsT=onesb.ap, rhs=sq.ap[:, c, :], start=(c == 0), stop=(c == 7)), reads=[onesb, sq], writes=[psm])
            kb.op("act", lambda e: e.activation(out=rb.ap, in_=psm.ap[:, 0:JB], func=AF.Sqrt, scale=1.0 / D, bias=epsb.ap[:, 0:1]), reads=[psm, epsb], writes=[rb])
            kb.op("dve", lambda e: e.reciprocal(out=rb.ap, in_=rb.ap), reads=[rb], writes=[rb])
            for c in range(8):
                kb.op("dve", lambda e, c=c, st=st, q=q: e.scalar_tensor_tensor(out=hT.ap[:, c, q * JB:(q + 1) * JB], in0=st.ap[:, c, :], scalar=g2.ap[:, c:c + 1],
                                                                           in1=rb.ap, op0=ALU.mult, op1=ALU.mult), reads=[st, g2, rb], writes=[hT])
        for ex in range(NEXP):
            for jb in range(NJB):
                w = ctr["w"] % 2; ctr["w"] += 1
                for (src, dst) in ((wg_v[ex, :, :, jb * JB:(jb + 1) * JB], Wg[w]), (wu_v[ex, :, :, jb * JB:(jb + 1) * JB], Wu[w])):
                    st = stg[ctr["s"] % 3]; ctr["s"] += 1
                    kb.dma("sp", st.ap, src, writes=[st])
                    kb.op("pool", lambda e, st=st, dst=dst: e.tensor_copy(out=dst.ap, in_=st.ap), reads=[st], writes=[dst])
                st = stg[ctr["s"] % 3]; ctr["s"] += 1
                stv = st.ap.rearrange("p c j -> p (c j)").rearrange("p (jc n) -> p jc n", n=D)
                kb.dma("sp", stv, wd_v[ex, :, jb * (JB // 128):(jb + 1) * (JB // 128), :], writes=[st])
                kb.op("pool", lambda e, stv=stv, w=w: e.tensor_copy(out=Wd[w].ap, in_=stv), reads=[st], writes=[Wd[w]])
                for ck in range(TG // 512):
                    H = hid[ctr["h"] % 2]; ctr["h"] += 1
                    for jc in range(JB // 128):
                        gp = gps[ctr["g"] % 2]; up = ups[ctr["g"] % 2]; SG = sg[ctr["g"] % 2]; ctr["g"] += 1
                        for c in range(8):
                            kb.op("pe", lambda e, c=c, gp=gp, jc=jc: e.matmul(gp.ap, lhsT=Wg[w].ap[:, c, jc * 128:(jc + 1) * 128], rhs=hT.ap[:, c, ck * 512:(ck + 1) * 512],
                                                                         start=(c == 0), stop=(c == 7)), reads=[Wg[w], hT], writes=[gp])
                        for c in range(8):
                            kb.op("pe", lambda e, c=c, up=up, jc=jc: e.matmul(up.ap, lhsT=Wu[w].ap[:, c, jc * 128:(jc + 1) * 128], rhs=hT.ap[:, c, ck * 512:(ck + 1) * 512],
                                                                         start=(c == 0), stop=(c == 7)), reads=[Wu[w], hT], writes=[up])
                        kb.op("act", lambda e, gp=gp, SG=SG: e.activation(out=SG.ap, in_=gp.ap, func=AF.Silu), reads=[gp], writes=[SG])
                        kb.op("dve", lambda e, up=up, SG=SG, H=H, jc=jc: e.tensor_tensor(out=H.ap[:, jc, :], in0=up.ap, in1=SG.ap, op=ALU.mult), reads=[up, SG], writes=[H])
                    for tt in range(4):
                        tile = ck * 4 + tt
                        gtile = G * (TG // 128) + tile
                        for half in range(2):
                            yp = yps[ctr["y"] % 2]; ctr["y"] += 1
                            for jc in range(JB // 128):
                                kb.op("pe", lambda e, jc=jc, yp=yp, half=half, H=H, tt=tt: e.matmul(yp.ap, lhsT=H.ap[:, jc, tt * 128:(tt + 1) * 128], rhs=Wd[w].ap[:, jc, half * 512:(half + 1) * 512],
                                                                                          start=(jc == 0), stop=(jc == JB // 128 - 1)), reads=[H, Wd[w]], writes=[yp])
                            kb.op("dve", lambda e, yp=yp, half=half, tile=tile, gtile=gtile, ex=ex: e.scalar_tensor_tensor(
                                out=XA[:, tile, half * 512:(half + 1) * 512], in0=yp.ap, scalar=GT.ap[:, gtile, ex:ex + 1],
                                in1=XA[:, tile, half * 512:(half + 1) * 512], op0=ALU.mult, op1=ALU.add), reads=[yp, GT, big], writes=[big])
        for tile in range(TG // 128):
            r0 = tok0 + tile * 128
            if not final:
                kb.dma("pool", xo[r0:r0 + 128, :], XA[:, tile, :], reads=[big], is_output=True)
            else:
                F_, FS = fin[tile % 2], fs[tile % 2]
                kb.op("act", lambda e, tile=tile, FS=FS: e.activation(out=junk.ap, in_=XA[:, tile, :], func=AF.Square, accum_out=FS.ap[:, 0:1]), reads=[big], writes=[junk, FS])
                kb.op("act", lambda e, FS=FS: e.activation(out=FS.ap[:, 0:1], in_=FS.ap[:, 0:1], func=AF.Sqrt, scale=1.0 / D, bias=epsb.ap[:, 0:1]), reads=[FS, epsb], writes=[FS])
                kb.op("dve", lambda e, FS=FS: e.reciprocal(out=FS.ap[:, 0:1], in_=FS.ap[:, 0:1]), reads=[FS], writes=[FS])
                kb.op("dve", lambda e, tile=tile, F_=F_, FS=FS: e.scalar_tensor_tensor(out=F_.ap, in0=XA[:, tile, :], scalar=FS.ap[:, 0:1], in1=gf.ap, op0=ALU.mult, op1=ALU.mult),
                      reads=[big, FS, gf], writes=[F_])
                kb.dma("pool", xo[r0:r0 + 128, :], F_.ap, reads=[F_], is_output=True)
    return kb


def run_l3(xmids, affs, norm_ffn_l, wg_l, wu_l, wd_l, final_norm, final):
    kb = build_l3(final)
    affP = np.concatenate([a[:SEG] for a in affs], 0)
    affS = np.concatenate([a[SEG:] for a in affs], 0)

    def lay(a):
        n = a.shape[0]
        return np.ascontiguousarray(a.T.reshape(NEXP, 8, n // 8).reshape(128, n // 8))

    p = np.arange(128)
    BD = (p[:, None] // 8 == p[None, :] // 8).astype(np.float32)
    Sel = (p[:, None] == 8 * np.arange(NEXP)[None, :]).astype(np.float32)
    base = {"affP": lay(affP), "affS": lay(affS), "BD": BD, "Sel": Sel,
            "g2": np.ascontiguousarray(norm_ffn_l.reshape(8, 128).T),
            "gfin": np.ascontiguousarray(np.broadcast_to(final_norm, (128, D))),
            "wg": np.ascontiguousarray(wg_l), "wu": np.ascontiguousarray(wu_l), "wd": np.ascontiguousarray(wd_l)}
    maps = []
    for c in range(NCORES):
        m = dict(base)
        m["xmT"] = np.ascontiguousarray(xmids[c].T)
        m["xmid"] = np.ascontiguousarray(xmids[c])
        m["affl"] = np.ascontiguousarray(affs[c])
        maps.append(m)
    res = _run(kb, maps)
    return [r["xo"] for r in res]


def kernel(x_prompt, x_sample, w_in, w_out, norm_mix, norm_ffn, q_gain, k_gain, out_gain, na_rpb, rel_bias,
           w_router, w_gate, w_up, w_down, final_norm):
    f = lambda a: np.asarray(a, dtype=np.float32)
    xp, xs = f(x_prompt)[0], f(x_sample)
    w_in, w_out, norm_mix, norm_ffn, q_gain, k_gain, out_gain = map(f, (w_in, w_out, norm_mix, norm_ffn, q_gain, k_gain, out_gain))
    na_rpb, rel_bias, w_router, w_gate, w_up, w_down, final_norm = map(f, (na_rpb, rel_bias, w_router, w_gate, w_up, w_down, final_norm))
    xc = [core_tokens(xp, xs, c) for c in range(NCORES)]
    for l in range(DEPTH):
        projs = run_l1(xc, w_in[l], norm_mix[l], q_gain[l], k_gain[l])
        xm, aff = run_l2(projs, xc, w_out[l], out_gain[l], norm_ffn[l], w_router[l], rel_bias, na_rpb[l])
        xc = run_l3(xm, aff, norm_ffn[l], w_gate[l], w_up[l], w_down[l], final_norm, final=(l == DEPTH - 1))
    y_prompt = np.concatenate([xc[c][:SEG] for c in range(NCORES)], 0)[None]
    y_sample = np.stack([xc[s // 4][SEG * (1 + s % 4):SEG * (2 + s % 4)] for s in range(NSEQ_S)], 0)
    return (np.ascontiguousarray(y_prompt, dtype=np.float32), np.ascontiguousarray(y_sample, dtype=np.float32))
```

```python
import numpy as np
import ml_dtypes
import concourse.bass as bass
import concourse.mybir as mybir
from concourse.bass_utils import run_bass_kernel_spmd

F32 = mybir.dt.float32
BF16 = mybir.dt.bfloat16
I32 = mybir.dt.int32
ALU = mybir.AluOpType
AF = mybir.ActivationFunctionType
AX = mybir.AxisListType
NPBF = ml_dtypes.bfloat16

NCORES = 8
D = 1024
DEPTH = 2
SEQ_P = 16384
NSEQ_S = 32
SEQ_S = 2048
SEG = 2048
NSEG = 5
NTOK = NSEG * SEG
NT = NTOK // 128
HD = 64
D_IN = 3840
EPS = 1e-6
NEXP = 16
DEXP = 2048
NEG = -30000.0


class Buf:
    def __init__(self, ap):
        self.ap = ap
        self.w = None
        self.r = []

    def __getitem__(self, k):
        return self.ap[k]


class KB:
    R = 4
    NDMA = 24

    def __init__(self):
        self.nc = bass.Bass("TRN2", target_bir_lowering=False)
        nc = self.nc
        self.eng = {"pe": nc.tensor, "act": nc.scalar, "dve": nc.vector,
                    "pool": nc.gpsimd, "sp": nc.sync}
        self.sems = {e: [nc.alloc_semaphore(f"s_{e}{i}") for i in range(self.R)]
                     for e in ("pe", "act", "dve", "pool")}
        self.cnt = {e: 0 for e in ("pe", "act", "dve", "pool")}
        self.dsem = [nc.alloc_semaphore(f"s_dma{i}") for i in range(self.NDMA)]
        self.dval = [0] * self.NDMA
        self.dnext = 0
        self.waited = {}
        self.out_tokens = []
        self.nbuf = 0

    def sb(self, shape, dt, name=None):
        self.nbuf += 1
        return Buf(self.nc.alloc_sbuf_tensor(name or f"sb{self.nbuf}", list(shape), dt).ap())

    def ps(self, shape, dt=F32, name=None):
        self.nbuf += 1
        return Buf(self.nc.alloc_psum_tensor(name or f"ps{self.nbuf}", list(shape), dt).ap())

    def dram(self, name, shape, dt, kind):
        return self.nc.dram_tensor(name, list(shape), dt, kind=kind).ap()

    def _wait(self, waiter, tok):
        if tok is None:
            return
        key, n = tok
        if self.waited.get((waiter, key), 0) >= n:
            return
        self.waited[(waiter, key)] = n
        e = self.eng[waiter]
        if key[0] == "dma":
            e.wait_ge(self.dsem[key[1]], n * 16)
        else:
            e.wait_ge(self.sems[key[0]][(n - 1) % self.R], (n - 1) // self.R + 1)

    def _deps(self, waiter, reads, writes):
        for b in reads:
            self._wait(waiter, b.w)
        for b in writes:
            self._wait(waiter, b.w)
            for t in b.r:
                self._wait(waiter, t)

    def _commit(self, tok, reads, writes):
        for b in reads:
            b.r.append(tok)
            if len(b.r) > 64:
                b.r = b.r[-64:]
        for b in writes:
            b.w = tok
            b.r = []

    def op(self, e, fn, reads=(), writes=()):
        self._deps(e, reads, writes)
        ins = fn(self.eng[e])
        self.cnt[e] += 1
        n = self.cnt[e]
        ins.then_inc(self.sems[e][(n - 1) % self.R], 1)
        tok = ((e,), n)
        self._commit(tok, reads, writes)
        return tok

    def dma(self, q, out, in_, reads=(), writes=(), is_output=False, **kw):
        self._deps(q, reads, writes)
        i = self.dnext
        self.dnext = (self.dnext + 1) % self.NDMA
        if self.dval[i] > 0:
            self._wait(q, (("dma", i), self.dval[i]))
        self.dval[i] += 1
        self.eng[q].dma_start(out=out, in_=in_, **kw).then_inc(self.dsem[i], 16)
        tok = (("dma", i), self.dval[i])
        self._commit(tok, reads, writes)
        if is_output:
            self.out_tokens.append(tok)
        return tok

    def finish(self):
        for t in self.out_tokens:
            self._wait("sp", t)
        return self.nc


def _run(kb, in_maps):
    nc = kb.finish()
    res = run_bass_kernel_spmd(nc, in_maps, core_ids=list(range(NCORES)))
    return res.results


def build_l1():
    kb = KB()
    nc = kb.nc
    xT = kb.dram("xT", [D, NTOK], F32, "ExternalInput")
    w_in = kb.dram("w_in", [D, D_IN], F32, "ExternalInput")
    gmix = kb.dram("gmix", [128, 8], F32, "ExternalInput")
    gqk = kb.dram("gqk", [128, 640], F32, "ExternalInput")
    cosd = kb.dram("cos", [NTOK, 32], F32, "ExternalInput")
    sind = kb.dram("sin", [NTOK, 32], F32, "ExternalInput")
    proj = kb.dram("proj", [NTOK, D_IN], BF16, "ExternalOutput")

    gW = kb.sb([128, 8, D_IN], BF16, "gW")
    gm = kb.sb([128, 8], F32, "gm")
    gq = kb.sb([128, 640], F32, "gq")
    ones = kb.sb([128, 1], BF16, "ones")
    kb.dma("sp", gm.ap, gmix, writes=[gm])
    kb.dma("sp", gq.ap, gqk, writes=[gq])
    kb.op("pool", lambda e: e.memset(ones.ap, 1.0), writes=[ones])
    epsb = kb.sb([128, 1], F32, "epsb")
    kb.op("pool", lambda e: e.memset(epsb.ap, EPS), writes=[epsb])
    wst = [kb.sb([128, 1920], F32, f"wst{i}") for i in range(2)]
    w_v = w_in.rearrange("(c p) n -> p c n", p=128)
    k = 0
    for c in range(8):
        for hf in range(2):
            st = wst[k % 2]
            k += 1
            kb.dma("sp", st.ap, w_v[:, c, hf * 1920:(hf + 1) * 1920], writes=[st])
            kb.op("dve", lambda e, st=st, c=c, hf=hf: e.tensor_scalar(
                out=gW.ap[:, c, hf * 1920:(hf + 1) * 1920], in0=st.ap, scalar1=gm.ap[:, c:c + 1],
                scalar2=None, op0=ALU.mult), reads=[st, gm], writes=[gW])

    NB = 2
    xs = [kb.sb([128, 8, 128], F32, f"xs{i}") for i in range(NB)]
    xb = [kb.sb([128, 8, 128], BF16, f"xb{i}") for i in range(NB)]
    sq = [kb.sb([128, 8, 128], BF16, f"sq{i}") for i in range(NB)]
    cs = [kb.sb([128, 32], F32, f"cs{i}") for i in range(NB)]
    sn = [kb.sb([128, 32], F32, f"sn{i}") for i in range(NB)]
    rstd = [kb.sb([128, 1], F32, f"rstd{i}") for i in range(NB)]
    ot = [kb.sb([128, 640], F32, f"ot{i}") for i in range(NB)]
    ob = [kb.sb([128, D_IN], BF16, f"ob{i}") for i in range(NB)]
    rs8 = [kb.sb([128, 1], F32, f"rs8{i}") for i in range(NB)]
    tA = [kb.sb([128, 640], F32, f"tA{i}") for i in range(NB)]
    tB = [kb.sb([128, 640], F32, f"tB{i}") for i in range(NB)]
    st10 = [kb.sb([128, 10], F32, f"st10{i}") for i in range(NB)]
    pss = kb.ps([128, 1], F32, "pss")
    psp = [kb.ps([128, 512], F32, f"psp{i}") for i in range(4)]
    xT_v = xT.rearrange("(c p) t -> p c t", p=128)
    colchunks = [(i * 512, min(512, D_IN - i * 512)) for i in range(8)]
    pk = 0
    CATS = [(0, 640, "a"), (640, 768, "p"), (768, 1536, "q"), (1536, 3072, "p"), (3072, 3328, "q"), (3328, 3840, "p")]
    for t in range(NT):
        b = t % NB
        X, XB, SQ, CS, SN, RS, OT, TA, TB, S10 = xs[b], xb[b], sq[b], cs[b], sn[b], rstd[b], ot[b], tA[b], tB[b], st10[b]
        OB, RS8 = ob[b], rs8[b]
        kb.dma("sp", X.ap, xT_v[:, :, t * 128:(t + 1) * 128], writes=[X])
        kb.dma("sp", CS.ap, cosd[t * 128:(t + 1) * 128, :], writes=[CS])
        kb.dma("sp", SN.ap, sind[t * 128:(t + 1) * 128, :], writes=[SN])
        kb.op("dve", lambda e: e.tensor_copy(out=XB.ap, in_=X.ap), reads=[X], writes=[XB])
        kb.op("act", lambda e: e.activation(out=SQ.ap, in_=X.ap, func=AF.Square), reads=[X], writes=[SQ])
        for c in range(8):
            kb.op("pe", lambda e, c=c: e.matmul(pss.ap, lhsT=SQ.ap[:, c, :], rhs=ones.ap, start=(c == 0), stop=(c == 7)),
                  reads=[SQ, ones], writes=[pss])
        kb.op("act", lambda e: e.activation(out=RS.ap, in_=pss.ap, func=AF.Sqrt, scale=1.0 / D, bias=epsb.ap[:, 0:1]),
              reads=[pss, epsb], writes=[RS])
        kb.op("dve", lambda e: e.reciprocal(out=RS.ap, in_=RS.ap), reads=[RS], writes=[RS])
        kb.op("dve", lambda e: e.tensor_scalar(out=RS8.ap, in0=RS.ap, scalar1=0.125, scalar2=None, op0=ALU.mult), reads=[RS], writes=[RS8])
        for (c0, w) in colchunks:
            P = psp[pk % 4]
            pk += 1
            for c in range(8):
                kb.op("pe", lambda e, c=c, P=P, c0=c0, w=w: e.matmul(P.ap[:, 0:w], lhsT=XB.ap[:, c, :], rhs=gW.ap[:, c, c0:c0 + w],
                                                               start=(c == 0), stop=(c == 7)), reads=[XB, gW], writes=[P])
            for (a0, a1, cat) in CATS:
                lo, hi = max(a0, c0), min(a1, c0 + w)
                if lo >= hi:
                    continue
                if cat == "a":
                    kb.op("act", lambda e, P=P, lo=lo, hi=hi, c0=c0: e.activation(out=OT.ap[:, lo:hi], in_=P.ap[:, lo - c0:hi - c0], func=AF.Copy, scale=RS.ap[:, 0:1]),
                          reads=[P, RS], writes=[OT])
                else:
                    SC = RS8 if cat == "q" else RS
                    kb.op("act", lambda e, P=P, lo=lo, hi=hi, c0=c0, SC=SC: e.activation(out=OB.ap[:, lo:hi], in_=P.ap[:, lo - c0:hi - c0], func=AF.Copy, scale=SC.ap[:, 0:1]),
                          reads=[P, SC], writes=[OB])
        y3 = OT.ap.rearrange("p (h d) -> p h d", d=64)
        ta3 = TA.ap.rearrange("p (h d) -> p h d", d=64)
        tb3 = TB.ap.rearrange("p (h d) -> p h d", d=64)
        kb.op("dve", lambda e: e.tensor_tensor(out=TA.ap, in0=OT.ap, in1=OT.ap, op=ALU.mult), reads=[OT], writes=[TA])
        kb.op("dve", lambda e: e.tensor_reduce(out=S10.ap, in_=ta3, axis=AX.X, op=ALU.add), reads=[TA], writes=[S10])
        kb.op("act", lambda e: e.activation(out=S10.ap, in_=S10.ap, func=AF.Sqrt, scale=1.0 / HD, bias=epsb.ap[:, 0:1]), reads=[S10, epsb], writes=[S10])
        kb.op("dve", lambda e: e.reciprocal(out=S10.ap, in_=S10.ap), reads=[S10], writes=[S10])
        kb.op("dve", lambda e: e.tensor_scalar(out=S10.ap[:, 0:8], in0=S10.ap[:, 0:8], scalar1=0.125, scalar2=None, op0=ALU.mult), reads=[S10], writes=[S10])
        kb.op("dve", lambda e: e.tensor_tensor(out=ta3, in0=y3, in1=S10.ap.unsqueeze(2).to_broadcast([128, 10, 64]), op=ALU.mult), reads=[OT, S10], writes=[TA])
        kb.op("dve", lambda e: e.tensor_tensor(out=TA.ap, in0=TA.ap, in1=gq.ap, op=ALU.mult), reads=[TA, gq], writes=[TA])
        x4 = TA.ap.rearrange("p (h i two) -> p h i two", i=32, two=2)
        o4 = OB.ap[:, 0:640].rearrange("p (h i two) -> p h i two", i=32, two=2)
        t4 = TB.ap.rearrange("p (h i two) -> p h i two", i=32, two=2)
        cb = CS.ap.unsqueeze(1).to_broadcast([128, 10, 32])
        sb_ = SN.ap.unsqueeze(1).to_broadcast([128, 10, 32])
        x1, x2 = x4[:, :, :, 0], x4[:, :, :, 1]
        kb.op("dve", lambda e: e.tensor_tensor(out=t4[:, :, :, 0], in0=x1, in1=cb, op=ALU.mult), reads=[TA, CS], writes=[TB])
        kb.op("dve", lambda e: e.tensor_tensor(out=t4[:, :, :, 1], in0=x2, in1=sb_, op=ALU.mult), reads=[TA, SN], writes=[TB])
        kb.op("dve", lambda e: e.tensor_tensor(out=o4[:, :, :, 0], in0=t4[:, :, :, 0], in1=t4[:, :, :, 1], op=ALU.subtract), reads=[TB], writes=[OB])
        kb.op("dve", lambda e: e.tensor_tensor(out=t4[:, :, :, 0], in0=x1, in1=sb_, op=ALU.mult), reads=[TA, SN], writes=[TB])
        kb.op("dve", lambda e: e.tensor_tensor(out=t4[:, :, :, 1], in0=x2, in1=cb, op=ALU.mult), reads=[TA, CS], writes=[TB])
        kb.op("dve", lambda e: e.tensor_tensor(out=o4[:, :, :, 1], in0=t4[:, :, :, 0], in1=t4[:, :, :, 1], op=ALU.add), reads=[TB], writes=[OB])
        kb.dma("pool", proj[t * 128:(t + 1) * 128, :], OB.ap, reads=[OB], is_output=True)
    return kb


def rope_tables(pos):
    row = (pos // 64).astype(np.float32)
    col = (pos % 64).astype(np.float32)
    half = HD // 2
    freqs = (np.float32(10000.0) ** (-np.arange(0, half, 2, dtype=np.float32) / np.float32(half))).astype(np.float32)
    ang = np.concatenate([row[:, None] * freqs, col[:, None] * freqs], axis=-1).astype(np.float32)
    return np.cos(ang).astype(np.float32), np.sin(ang).astype(np.float32)


def core_tokens(xp, xs, c):
    return np.concatenate([xp[c * SEG:(c + 1) * SEG]] + [xs[4 * c + i] for i in range(4)], axis=0)


def run_l1(xcores, w_in_l, norm_mix_l, q_gain_l, k_gain_l):
    kb = build_l1()
    gq = np.concatenate([np.tile(q_gain_l, 8), np.tile(k_gain_l, 2)]).astype(np.float32)
    maps = []
    for c in range(NCORES):
        pos = np.concatenate([np.arange(c * SEG, (c + 1) * SEG)] + [np.arange(SEG)] * 4)
        cs, sn = rope_tables(pos)
        maps.append({
            "xT": np.ascontiguousarray(xcores[c].T),
            "w_in": np.ascontiguousarray(w_in_l),
            "gmix": np.ascontiguousarray(norm_mix_l.reshape(8, 128).T),
            "gqk": np.ascontiguousarray(np.broadcast_to(gq, (128, 640))),
            "cos": cs, "sin": sn,
        })
    res = _run(kb, maps)
    return [r["proj"] for r in res]


PADSEG = 4096
HALO = 1024
B_GR = [(1, 64), (4, 256), (16, 1024)]
B_NT = [5, 8, 20]
B_SW = [128 * (n - 1) + 512 for n in B_NT]
B_SO = [0, B_SW[0], B_SW[0] + B_SW[1]]
B_STRIP = sum(B_SW)
C_NT = 8
NQB = NTOK // 512


def build_l2():
    kb = KB()
    QT = kb.dram("QT", [24, 64, NTOK], BF16, "ExternalInput")
    KTAp = kb.dram("KTAp", [2, 64, SEQ_P], BF16, "ExternalInput")
    VAp = kb.dram("VAp", [2, SEQ_P, 65], BF16, "ExternalInput")
    KTAs = kb.dram("KTAs", [2, 64, 4 * SEG], BF16, "ExternalInput")
    VAs = kb.dram("VAs", [2, 4 * SEG, 65], BF16, "ExternalInput")
    KTB = kb.dram("KTB", [12, 64, NSEG * PADSEG], BF16, "ExternalInput")
    VB = kb.dram("VB", [12, NSEG * PADSEG, 65], BF16, "ExternalInput")
    KTC = kb.dram("KTC", [4, 64, NSEG * PADSEG], BF16, "ExternalInput")
    VC = kb.dram("VC", [4, NSEG * PADSEG, 65], BF16, "ExternalInput")
    stripB = kb.dram("stripB", [128, 4, B_STRIP], F32, "ExternalInput")
    biasC = kb.dram("biasC", [5, 4, C_NT, 128, 512], F32, "ExternalInput")
    xin = kb.dram("x", [NTOK, D], F32, "ExternalInput")
    w_out = kb.dram("w_out", [D, D], F32, "ExternalInput")
    gout = kb.dram("gout", [64, 16], F32, "ExternalInput")
    g2d = kb.dram("g2", [128, 8], F32, "ExternalInput")
    w_r = kb.dram("w_r", [D, NEXP], F32, "ExternalInput")
    xmid = kb.dram("xmid", [NTOK, D], F32, "ExternalOutput")
    affo = kb.dram("aff", [NTOK, NEXP], F32, "ExternalOutput")

    stage = kb.sb([128, 4096], F32, "stage")
    sB = kb.sb([128, 4, B_STRIP], BF16, "sB")
    gWo = kb.sb([64, 16, D], BF16, "gWo")
    go = kb.sb([64, 16], F32, "go")
    g2 = kb.sb([128, 8], F32, "g2sb")
    identb = kb.sb([128, 128], BF16, "identb")
    identf = kb.sb([128, 128], F32, "identf")
    onesb = kb.sb([128, 64], BF16, "onesb")
    epsb = kb.sb([128, 1], F32, "epsb")
    gw32 = kb.sb([128, 8, NEXP], F32, "gw32")
    gwh = kb.sb([128, 8, NEXP], BF16, "gwh")
    gwl = kb.sb([128, 8, NEXP], BF16, "gwl")
    kb.op("pool", lambda e: e.memset(identf.ap, 0.0), writes=[identf])
    kb.op("pool", lambda e: e.affine_select(out=identf.ap, in_=identf.ap, pattern=[[-1, 128]], compare_op=ALU.not_equal,
                                            fill=1.0, base=0, channel_multiplier=1), reads=[identf], writes=[identf])
    kb.op("dve", lambda e: e.tensor_copy(out=identb.ap, in_=identf.ap), reads=[identf], writes=[identb])
    kb.op("pool", lambda e: e.memset(onesb.ap, 1.0), writes=[onesb])
    kb.op("pool", lambda e: e.memset(epsb.ap, EPS), writes=[epsb])
    kb.dma("sp", go.ap, gout, writes=[go])
    kb.dma("sp", g2.ap, g2d, writes=[g2])
    for h in range(4):
        for (o, w) in ((0, 4096), (4096, B_STRIP - 4096)):
            kb.dma("sp", stage.ap[:, 0:w], stripB[:, h, o:o + w], writes=[stage])
            kb.op("dve", lambda e, h=h, o=o, w=w: e.tensor_copy(out=sB.ap[:, h, o:o + w], in_=stage.ap[:, 0:w]), reads=[stage], writes=[sB])
    wo_v = w_out.rearrange("(s d) n -> d s n", d=64)
    for s4 in range(4):
        stv = stage.ap[0:64, :].rearrange("p (s n) -> p s n", s=4)
        kb.dma("sp", stv, wo_v[:, s4 * 4:(s4 + 1) * 4, :], writes=[stage])
        for s in range(4):
            sl = s4 * 4 + s
            kb.op("dve", lambda e, s=s, sl=sl, stv=stv: e.tensor_scalar(out=gWo.ap[:, sl, :], in0=stv[:, s, :], scalar1=go.ap[:, sl:sl + 1],
                                                                        scalar2=None, op0=ALU.mult), reads=[stage, go], writes=[gWo])
    kb.dma("sp", gw32.ap, w_r.rearrange("(c p) e -> p c e", p=128), writes=[gw32])
    kb.op("dve", lambda e: e.tensor_tensor(out=gw32.ap, in0=gw32.ap, in1=g2.ap.unsqueeze(2).to_broadcast([128, 8, NEXP]), op=ALU.mult),
          reads=[gw32, g2], writes=[gw32])
    kb.op("dve", lambda e: e.tensor_copy(out=gwh.ap, in_=gw32.ap), reads=[gw32], writes=[gwh])
    kb.op("dve", lambda e: e.tensor_tensor(out=gwl.ap, in0=gw32.ap, in1=gwh.ap, op=ALU.subtract), reads=[gw32, gwh], writes=[gwl])

    qbuf = [kb.sb([64, 4, 512], BF16, f"qbuf{i}") for i in range(2)]
    kbuf = [kb.sb([64, 2560], BF16, f"kbuf{i}") for i in range(2)]
    vbuf = [kb.sb([128, 20, 65], BF16, f"vbuf{i}") for i in range(2)]
    bcb = [kb.sb([128, C_NT, 512], BF16, f"bcb{i}") for i in range(2)]
    PT = [kb.sb([128, 512], BF16, f"PT{i}") for i in range(3)]
    mixT = kb.sb([64, 16, 512], BF16, "mixT")
    osq = kb.sb([64, 16, 128], BF16, "osq")
    rden = kb.sb([128, 512], F32, "rden")
    rh = kb.sb([128, 512], BF16, "rh")
    rl = kb.sb([128, 512], BF16, "rl")
    bcs = kb.sb([64, 512], F32, "bcs")
    xm = [kb.sb([128, D], F32, f"xm{i}") for i in range(2)]
    xh = kb.sb([128, 8, 128], BF16, "xh")
    xl = kb.sb([128, 8, 128], BF16, "xl")
    junk = kb.sb([128, D], BF16, "junk")
    sm = [kb.sb([128, 8], F32, f"sm{i}") for i in range(2)]
    lg = [kb.sb([128, NEXP], F32, f"lg{i}") for i in range(2)]
    acc = [kb.ps([128, 512], F32, f"acc{i}") for i in range(4)]
    stp = [kb.ps([128, 512], F32, f"stp{i}") for i in range(2)]
    bcp = kb.ps([128, 512], F32, "bcp")
    cnt = {"k": 0, "q": 0, "s": 0, "p": 0, "c": 0}

    def nxt(name, lst):
        b = lst[cnt[name] % len(lst)]
        cnt[name] += 1
        return b

    def score_pv(K, koff, Q, qs, V, vt, A, first, last, bias=None):
        S = nxt("s", stp)
        P = nxt("p", PT)
        kb.op("pe", lambda e: e.matmul(S.ap, lhsT=K.ap[:, koff:koff + 128], rhs=Q.ap[:, qs, :], start=True, stop=(bias is None)),
              reads=[K, Q], writes=[S])
        if bias is not None:
            bbuf, bap = bias
            kb.op("pe", lambda e: e.matmul(S.ap, lhsT=identb.ap, rhs=bap, start=False, stop=True), reads=[identb, bbuf], writes=[S])
        kb.op("act", lambda e: e.activation(out=P.ap, in_=S.ap, func=AF.Exp), reads=[S], writes=[P])
        kb.op("pe", lambda e: e.matmul(A.ap[0:65, :], lhsT=V.ap[:, vt, :], rhs=P.ap, start=first, stop=last), reads=[V, P], writes=[A])

    def finalize(A, slot):
        kb.op("dve", lambda e: e.reciprocal(out=rden.ap[64:65, :], in_=A.ap[64:65, :]), reads=[A], writes=[rden])
        kb.op("dve", lambda e: e.tensor_copy(out=rh.ap[64:65, :], in_=rden.ap[64:65, :]), reads=[rden], writes=[rh])
        kb.op("dve", lambda e: e.tensor_tensor(out=rl.ap[64:65, :], in0=rden.ap[64:65, :], in1=rh.ap[64:65, :], op=ALU.subtract),
              reads=[rden, rh], writes=[rl])
        kb.op("pe", lambda e: e.matmul(bcp.ap[0:64, :], lhsT=onesb.ap[64:65, 0:64], rhs=rh.ap[64:65, :], start=True, stop=False),
              reads=[onesb, rh], writes=[bcp])
        kb.op("pe", lambda e: e.matmul(bcp.ap[0:64, :], lhsT=onesb.ap[64:65, 0:64], rhs=rl.ap[64:65, :], start=False, stop=True),
              reads=[onesb, rl], writes=[bcp])
        kb.op("act", lambda e: e.activation(out=bcs.ap, in_=bcp.ap[0:64, :], func=AF.Copy), reads=[bcp], writes=[bcs])
        kb.op("dve", lambda e: e.tensor_tensor(out=mixT.ap[:, slot, :], in0=A.ap[0:64, :], in1=bcs.ap, op=ALU.mult), reads=[A, bcs], writes=[mixT])

    def load_kv(KTd, Vd, col0, ntile):
        K = nxt("k", kbuf)
        V = vbuf[(cnt["k"] - 1) % 2]
        kb.dma("sp", K.ap[:, 0:ntile * 128], KTd[:, col0:col0 + ntile * 128], writes=[K])
        kb.dma("sp", V.ap[:, 0:ntile, :], Vd[col0:col0 + ntile * 128, :].rearrange("(t p) c -> p t c", p=128), writes=[V])
        return K, V

    for qb in range(NQB):
        seg, qbl = qb // 4, qb % 4
        t0 = qb * 512
        for j in range(2):
            Q = nxt("q", qbuf)
            kb.dma("sp", Q.ap, QT[4 * j:4 * j + 4, :, t0:t0 + 512].rearrange("s d t -> d s t"), writes=[Q])
            if seg == 0:
                chunks = [(KTAp[j], VAp[j], kc * 2048) for kc in range(8)]
            else:
                chunks = [(KTAs[j], VAs[j], (seg - 1) * SEG)]
            for ci, (KTd, Vd, col0) in enumerate(chunks):
                K, V = load_kv(KTd, Vd, col0, 16)
                for kt in range(16):
                    first = (ci == 0 and kt == 0)
                    last = (ci == len(chunks) - 1 and kt == 15)
                    for qh in range(4):
                        score_pv(K, kt * 128, Q, qh, V, kt, acc[qh], first, last)
            for qh in range(4):
                finalize(acc[qh], 4 * j + qh)
        for h in range(4):
            Q = nxt("q", qbuf)
            kb.dma("sp", Q.ap[:, 0:3, :], QT[8 + h:20:4, :, t0:t0 + 512].rearrange("s d t -> d s t"), writes=[Q])
            A = acc[h]
            for g in range(3):
                reach, nt = B_GR[g][1], B_NT[g]
                col0 = seg * PADSEG + HALO + 512 * qbl - reach
                K, V = load_kv(KTB[g * 4 + h], VB[g * 4 + h], col0, nt)
                for kt in range(nt):
                    so = B_SO[g] + 128 * (nt - 1 - kt)
                    score_pv(K, kt * 128, Q, g, V, kt, A, (g == 0 and kt == 0), (g == 2 and kt == nt - 1),
                             bias=(sB, sB.ap[:, h, so:so + 512]))
            finalize(A, 8 + h)
        if seg == 0:
            cls = {0: 3, 3: 4}.get(qbl, 1)
        else:
            cls = {0: 0, 3: 2}.get(qbl, 1)
        for h in range(4):
            Q = nxt("q", qbuf)
            kb.dma("sp", Q.ap[:, 0:1, :], QT[20 + h:21 + h, :, t0:t0 + 512].rearrange("s d t -> d s t"), writes=[Q])
            A = acc[h]
            col0 = seg * PADSEG + HALO + 512 * qbl - 256
            K, V = load_kv(KTC[h], VC[h], col0, C_NT)
            BC = nxt("c", bcb)
            stv = stage.ap.rearrange("p (k n) -> p k n", k=C_NT)
            kb.dma("sp", stv, biasC[cls, h].rearrange("k p n -> p k n"), writes=[stage])
            kb.op("dve", lambda e, BC=BC, stv=stv: e.tensor_copy(out=BC.ap, in_=stv), reads=[stage], writes=[BC])
            for kt in range(C_NT):
                score_pv(K, kt * 128, Q, 0, V, kt, A, kt == 0, kt == C_NT - 1, bias=(BC, BC.ap[:, kt, :]))
            finalize(A, 12 + h)
        for tt in range(4):
            tok0 = t0 + tt * 128
            X = xm[tt % 2]
            SM = sm[tt % 2]
            LG = lg[tt % 2]
            kb.dma("sp", X.ap, xin[tok0:tok0 + 128, :], writes=[X])
            kb.op("dve", lambda e: e.tensor_tensor(out=osq.ap, in0=mixT.ap[:, :, tt * 128:(tt + 1) * 128], in1=mixT.ap[:, :, tt * 128:(tt + 1) * 128], op=ALU.mult),
                  reads=[mixT], writes=[osq])
            for m, (s0, s1, width) in enumerate(((0, 8, 512), (8, 12, 256), (12, 16, 256))):
                for s in range(s0, s1):
                    kb.op("pe", lambda e, s=s: e.matmul(acc[2].ap[:, 0:1], lhsT=osq.ap[:, s, :], rhs=onesb.ap[0:64, 0:1], start=(s == s0), stop=(s == s1 - 1)),
                          reads=[osq, onesb], writes=[acc[2]])
                kb.op("act", lambda e, m=m, width=width: e.activation(out=SM.ap[:, m:m + 1], in_=acc[2].ap[:, 0:1], func=AF.Sqrt, scale=1.0 / width, bias=epsb.ap[:, 0:1]),
                      reads=[acc[2], epsb], writes=[SM])
                kb.op("dve", lambda e, m=m: e.reciprocal(out=SM.ap[:, m:m + 1], in_=SM.ap[:, m:m + 1]), reads=[SM], writes=[SM])
                for half in range(2):
                    O = acc[half]
                    for s in range(s0, s1):
                        kb.op("pe", lambda e, s=s, O=O, half=half: e.matmul(O.ap, lhsT=mixT.ap[:, s, tt * 128:(tt + 1) * 128], rhs=gWo.ap[:, s, half * 512:(half + 1) * 512],
                                                                       start=(s == s0), stop=(s == s1 - 1)), reads=[mixT, gWo], writes=[O])
                    kb.op("dve", lambda e, O=O, half=half, m=m: e.scalar_tensor_tensor(out=X.ap[:, half * 512:(half + 1) * 512], in0=O.ap, scalar=SM.ap[:, m:m + 1],
                                                                                     in1=X.ap[:, half * 512:(half + 1) * 512], op0=ALU.mult, op1=ALU.add),
                          reads=[O, SM, X], writes=[X])
            kb.dma("pool", xmid[tok0:tok0 + 128, :], X.ap, reads=[X], is_output=True)
            kb.op("act", lambda e: e.activation(out=junk.ap, in_=X.ap, func=AF.Square, accum_out=SM.ap[:, 3:4]), reads=[X], writes=[junk, SM])
            kb.op("act", lambda e: e.activation(out=SM.ap[:, 3:4], in_=SM.ap[:, 3:4], func=AF.Sqrt, scale=1.0 / D, bias=epsb.ap[:, 0:1]), reads=[SM, epsb], writes=[SM])
            kb.op("dve", lambda e: e.reciprocal(out=SM.ap[:, 3:4], in_=SM.ap[:, 3:4]), reads=[SM], writes=[SM])
            for c in range(8):
                kb.op("pe", lambda e, c=c: e.transpose(acc[3].ap[:, (c % 4) * 128:(c % 4 + 1) * 128], X.ap[:, c * 128:(c + 1) * 128], identf.ap),
                      reads=[X, identf], writes=[acc[3]])
                kb.op("act", lambda e, c=c: e.activation(out=xh.ap[:, c, :], in_=acc[3].ap[:, (c % 4) * 128:(c % 4 + 1) * 128], func=AF.Copy), reads=[acc[3]], writes=[xh])
                kb.op("dve", lambda e, c=c: e.tensor_tensor(out=xl.ap[:, c, :], in0=acc[3].ap[:, (c % 4) * 128:(c % 4 + 1) * 128], in1=xh.ap[:, c, :], op=ALU.subtract),
                      reads=[acc[3], xh], writes=[xl])
            k = 0
            for c in range(8):
                for (xa, wa) in ((xh, gwh), (xh, gwl), (xl, gwh)):
                    kb.op("pe", lambda e, c=c, xa=xa, wa=wa, k=k: e.matmul(acc[2].ap[:, 16:32], lhsT=xa.ap[:, c, :], rhs=wa.ap[:, c, :], start=(k == 0), stop=(k == 23)),
                          reads=[xa, wa], writes=[acc[2]])
                    k += 1
            kb.op("act", lambda e: e.activation(out=LG.ap, in_=acc[2].ap[:, 16:32], func=AF.Copy, scale=SM.ap[:, 3:4]), reads=[acc[2], SM], writes=[LG])
            kb.op("dve", lambda e: e.tensor_reduce(out=SM.ap[:, 4:5], in_=LG.ap, axis=AX.X, op=ALU.max), reads=[LG], writes=[SM])
            kb.op("dve", lambda e: e.tensor_scalar(out=SM.ap[:, 4:5], in0=SM.ap[:, 4:5], scalar1=-1.0, scalar2=None, op0=ALU.mult), reads=[SM], writes=[SM])
            kb.op("act", lambda e: e.activation(out=LG.ap, in_=LG.ap, func=AF.Exp, bias=SM.ap[:, 4:5], accum_out=SM.ap[:, 5:6]), reads=[LG, SM], writes=[LG, SM])
            kb.op("dve", lambda e: e.reciprocal(out=SM.ap[:, 5:6], in_=SM.ap[:, 5:6]), reads=[SM], writes=[SM])
            kb.op("dve", lambda e: e.tensor_scalar(out=LG.ap, in0=LG.ap, scalar1=SM.ap[:, 5:6], scalar2=None, op0=ALU.mult), reads=[LG, SM], writes=[LG])
            kb.dma("pool", affo[tok0:tok0 + 128, :], LG.ap, reads=[LG], is_output=True)
    return kb


def _t5_bucket(rel):
    nb, max_exact = 16, 8
    ret = (rel > 0).astype(np.int32) * nb
    n = np.abs(rel)
    lg = np.log(np.maximum(n, 1).astype(np.float32) / np.float32(max_exact)) / np.float32(np.log(2048.0 / max_exact)) * np.float32(nb - max_exact)
    large = np.minimum(max_exact + lg.astype(np.float32).astype(np.int32), nb - 1)
    return ret + np.where(n < max_exact, n, large)


def _strip_b(rel_bias):
    out = np.full((128, 4, B_STRIP), NEG, np.float32)
    i = np.arange(128)[:, None]
    for g, (d, reach) in enumerate(B_GR):
        nt, w = B_NT[g], B_SW[g]
        m = np.arange(w)[None, :]
        rel = i - m + 128 * (nt - 1) - reach
        valid = (rel % d == 0) & (np.abs(rel) <= 64 * d)
        bk = _t5_bucket(rel)
        for h in range(4):
            vals = rel_bias[bk, g * 4 + h]
            out[:, h, B_SO[g]:B_SO[g] + w] = np.where(valid, vals, np.float32(NEG))
    return out


def _bias_c(rpb, R, rows):
    out = np.full((4, C_NT, 128, 512), NEG, np.float32)
    r = (8 * R + np.arange(8))[:, None].repeat(64, 1).reshape(-1)
    c = np.tile(np.arange(64), 8)
    rs = np.clip(r - 4, 0, rows - 8)
    cs = np.clip(c - 8, 0, 64 - 16)
    for kt in range(C_NT):
        kr = (8 * R - 4 + 2 * kt + np.arange(2))[:, None].repeat(64, 1).reshape(-1)
        ck = np.tile(np.arange(64), 2)
        valid = ((kr[:, None] >= rs[None, :]) & (kr[:, None] <= rs[None, :] + 7) & (kr[:, None] >= 0) & (kr[:, None] < rows)
                 & (ck[:, None] >= cs[None, :]) & (ck[:, None] < cs[None, :] + 16))
        dr = np.clip(kr[:, None] - r[None, :] + 7, 0, 14)
        dc = np.clip(ck[:, None] - c[None, :], -15, 15) + 15
        for h in range(4):
            out[h, kt] = np.where(valid, rpb[h][dr, dc], np.float32(NEG))
    return out


def _aug(v, valid):
    return np.concatenate([v, valid[:, None].astype(v.dtype)], axis=1)


def run_l2(projs, xcores, w_out_l, out_gain_l, norm_ffn_l, w_r_l, rel_bias, na_rpb_l):
    kb = build_l2()
    Pp = np.concatenate([projs[c][:SEG] for c in range(NCORES)], 0)
    one_p = np.ones(SEQ_P, bool)
    KTAp = np.ascontiguousarray(np.stack([Pp[:, 512 + 64 * j:576 + 64 * j].T for j in range(2)]))
    VAp = np.ascontiguousarray(np.stack([_aug(Pp[:, 640 + 64 * j:704 + 64 * j], one_p) for j in range(2)]))
    stripB = _strip_b(rel_bias)
    bc_first, bc_mid, bc_last = _bias_c(na_rpb_l, 0, 32), _bias_c(na_rpb_l, 1, 32), _bias_c(na_rpb_l, 3, 32)
    qcols = [64 * s for s in range(8)] + [768 + 64 * k for k in range(12)] + [3072 + 64 * h for h in range(4)]
    maps = []
    for c in range(NCORES):
        loc = projs[c]
        QT = np.ascontiguousarray(np.stack([loc[:, q:q + 64].T for q in qcols]))
        ls = loc[SEG:]
        one_s = np.ones(4 * SEG, bool)
        KTAs = np.ascontiguousarray(np.stack([ls[:, 512 + 64 * j:576 + 64 * j].T for j in range(2)]))
        VAs = np.ascontiguousarray(np.stack([_aug(ls[:, 640 + 64 * j:704 + 64 * j], one_s) for j in range(2)]))
        pad = np.zeros((NSEG * PADSEG, D_IN), loc.dtype)
        val = np.zeros(NSEG * PADSEG, bool)
        g0, g1 = max(0, SEG * c - HALO), min(SEQ_P, SEG * c + SEG + HALO)
        o0 = g0 - (SEG * c - HALO)
        pad[o0:o0 + (g1 - g0)] = Pp[g0:g1]
        val[o0:o0 + (g1 - g0)] = True
        for s in range(1, NSEG):
            pad[s * PADSEG + HALO:s * PADSEG + HALO + SEG] = loc[s * SEG:(s + 1) * SEG]
            val[s * PADSEG + HALO:s * PADSEG + HALO + SEG] = True
        KTB = np.ascontiguousarray(np.stack([pad[:, 1536 + 64 * k:1600 + 64 * k].T for k in range(12)]))
        VB = np.ascontiguousarray(np.stack([_aug(pad[:, 2304 + 64 * k:2368 + 64 * k], val) for k in range(12)]))
        KTC = np.ascontiguousarray(np.stack([pad[:, 3328 + 64 * h:3392 + 64 * h].T for h in range(4)]))
        VC = np.ascontiguousarray(np.stack([_aug(pad[:, 3584 + 64 * h:3648 + 64 * h], val) for h in range(4)]))
        biasC = np.stack([bc_first, bc_mid, bc_last, _bias_c(na_rpb_l, 4 * c, 256), _bias_c(na_rpb_l, 4 * c + 3, 256)])
        maps.append({
            "QT": QT, "KTAp": KTAp, "VAp": VAp, "KTAs": KTAs, "VAs": VAs, "KTB": KTB, "VB": VB, "KTC": KTC, "VC": VC,
            "stripB": stripB, "biasC": np.ascontiguousarray(biasC), "x": np.ascontiguousarray(xcores[c]),
            "w_out": np.ascontiguousarray(w_out_l), "gout": np.ascontiguousarray(out_gain_l.reshape(16, 64).T),
            "g2": np.ascontiguousarray(norm_ffn_l.reshape(8, 128).T), "w_r": np.ascontiguousarray(w_r_l),
        })
    res = _run(kb, maps)
    return [r["xmid"] for r in res], [r["aff"] for r in res]


TG = 2048
NG = NTOK // TG
JB = 256
NJB = DEXP // JB
NBIS = 34


def build_l3(final):
    kb = KB()
    xmT = kb.dram("xmT", [D, NTOK], F32, "ExternalInput")
    xmid = kb.dram("xmid", [NTOK, D], F32, "ExternalInput")
    affl = kb.dram("affl", [NTOK, NEXP], F32, "ExternalInput")
    affP = kb.dram("affP", [128, SEQ_P // 8], F32, "ExternalInput")
    affS = kb.dram("affS", [128, NSEQ_S * SEQ_S // 8], F32, "ExternalInput")
    BDd = kb.dram("BD", [128, 128], F32, "ExternalInput")
    Seld = kb.dram("Sel", [128, NEXP], F32, "ExternalInput")
    g2d = kb.dram("g2", [128, 8], F32, "ExternalInput")
    gfd = kb.dram("gfin", [128, D], F32, "ExternalInput")
    wg = kb.dram("wg", [NEXP, D, DEXP], F32, "ExternalInput")
    wu = kb.dram("wu", [NEXP, D, DEXP], F32, "ExternalInput")
    wd = kb.dram("wd", [NEXP, DEXP, D], F32, "ExternalInput")
    xo = kb.dram("xo", [NTOK, D], F32, "ExternalOutput")

    big = kb.sb([128, 16384], F32, "big")
    AS = big.ap[:, 0:8192]
    MK = big.ap[:, 8192:12288].bitcast(BF16)
    APv = big.ap[:, 12288:14336]
    BD32 = kb.sb([128, 128], F32, "BD32")
    BD = kb.sb([128, 128], BF16, "BDb")
    Sel = kb.sb([128, NEXP], F32, "Selb")
    g2 = kb.sb([128, 8], F32, "g2sb")
    gf = kb.sb([128, D], F32, "gfsb")
    onesb = kb.sb([128, 128], BF16, "onesb")
    epsb = kb.sb([128, 1], F32, "epsb")
    AL = kb.sb([128, NT, NEXP], F32, "AL")
    GT = kb.sb([128, NT, NEXP], F32, "GT")
    small = {n: kb.sb([128, 1], F32, "sm_" + n) for n in ("lo", "hi", "md", "c1", "cl32", "ge", "nge", "t1", "t2", "rem", "pf")}
    chb = kb.sb([128, 1], BF16, "chb")
    clb = kb.sb([128, 1], BF16, "clb")
    pb = kb.sb([128, 1], BF16, "pb")
    R32 = kb.sb([128, NEXP], F32, "R32")
    Rb = [kb.sb([128, NEXP], BF16, f"Rb{i}") for i in range(3)]
    Tthr = [kb.sb([128, NEXP], F32, f"Tthr{i}") for i in range(2)]
    psm = kb.ps([128, 512], F32, "psm")
    kb.op("pool", lambda e: e.memset(onesb.ap, 1.0), writes=[onesb])
    kb.op("pool", lambda e: e.memset(epsb.ap, EPS), writes=[epsb])
    kb.dma("sp", BD32.ap, BDd, writes=[BD32])
    kb.op("dve", lambda e: e.tensor_copy(out=BD.ap, in_=BD32.ap), reads=[BD32], writes=[BD])
    kb.dma("sp", Sel.ap, Seld, writes=[Sel])
    kb.dma("sp", g2.ap, g2d, writes=[g2])
    kb.dma("sp", gf.ap, gfd, writes=[gf])
    kb.dma("sp", AL.ap, affl.rearrange("(t p) e -> p t e", p=128), writes=[AL])
    kb.dma("sp", AS, affS, writes=[big])
    kb.dma("sp", APv, affP, writes=[big])
    S = small

    def dve(fn, reads, writes):
        return kb.op("dve", fn, reads=reads, writes=writes)

    for gi, (Abuf, Aap, n, cap) in enumerate(((big, APv, SEQ_P // 8, SEQ_P // 8), (big, AS, NSEQ_S * SEQ_S // 8, NSEQ_S * SEQ_S // 8))):
        dve(lambda e: e.memset(S["lo"].ap, 0.0), [], [S["lo"]])
        dve(lambda e: e.memset(S["hi"].ap, 1.0), [], [S["hi"]])
        for it in range(NBIS):
            dve(lambda e: e.tensor_tensor(out=S["md"].ap, in0=S["lo"].ap, in1=S["hi"].ap, op=ALU.add), [S["lo"], S["hi"]], [S["md"]])
            dve(lambda e: e.tensor_scalar(out=S["md"].ap, in0=S["md"].ap, scalar1=0.5, scalar2=None, op0=ALU.mult), [S["md"]], [S["md"]])
            dve(lambda e: e.tensor_scalar(out=MK[:, 0:n], in0=Aap, scalar1=S["md"].ap[:, 0:1], scalar2=None, op0=ALU.is_ge), [Abuf, S["md"]], [big])
            dve(lambda e: e.tensor_reduce(out=S["c1"].ap, in_=MK[:, 0:n], axis=AX.X, op=ALU.add), [big], [S["c1"]])
            dve(lambda e: e.tensor_copy(out=chb.ap, in_=S["c1"].ap), [S["c1"]], [chb])
            dve(lambda e: e.tensor_tensor(out=clb.ap, in0=S["c1"].ap, in1=chb.ap, op=ALU.subtract), [S["c1"], chb], [clb])
            kb.op("pe", lambda e: e.matmul(psm.ap[:, 0:1], lhsT=BD.ap, rhs=chb.ap, start=True, stop=False), reads=[BD, chb], writes=[psm])
            kb.op("pe", lambda e: e.matmul(psm.ap[:, 0:1], lhsT=BD.ap, rhs=clb.ap, start=False, stop=True), reads=[BD, clb], writes=[psm])
            dve(lambda e: e.tensor_scalar(out=S["ge"].ap, in0=psm.ap[:, 0:1], scalar1=float(cap), scalar2=None, op0=ALU.is_ge), [psm], [S["ge"]])
            dve(lambda e: e.tensor_scalar(out=S["nge"].ap, in0=S["ge"].ap, scalar1=-1.0, scalar2=1.0, op0=ALU.mult, op1=ALU.add), [S["ge"]], [S["nge"]])
            dve(lambda e: e.tensor_tensor(out=S["t1"].ap, in0=S["lo"].ap, in1=S["nge"].ap, op=ALU.mult), [S["lo"], S["nge"]], [S["t1"]])
            dve(lambda e: e.scalar_tensor_tensor(out=S["lo"].ap, in0=S["md"].ap, scalar=S["ge"].ap[:, 0:1], in1=S["t1"].ap, op0=ALU.mult, op1=ALU.add),
                [S["md"], S["ge"], S["t1"]], [S["lo"]])
            dve(lambda e: e.tensor_tensor(out=S["t2"].ap, in0=S["hi"].ap, in1=S["ge"].ap, op=ALU.mult), [S["hi"], S["ge"]], [S["t2"]])
            dve(lambda e: e.scalar_tensor_tensor(out=S["hi"].ap, in0=S["md"].ap, scalar=S["nge"].ap[:, 0:1], in1=S["t2"].ap, op0=ALU.mult, op1=ALU.add),
                [S["md"], S["nge"], S["t2"]], [S["hi"]])
        dve(lambda e: e.tensor_copy(out=S["rem"].ap, in_=S["lo"].ap), [S["lo"]], [S["rem"]])
        for k in range(3):
            dve(lambda e: e.tensor_copy(out=pb.ap, in_=S["rem"].ap), [S["rem"]], [pb])
            dve(lambda e: e.tensor_copy(out=S["pf"].ap, in_=pb.ap), [pb], [S["pf"]])
            dve(lambda e: e.tensor_tensor(out=S["rem"].ap, in0=S["rem"].ap, in1=S["pf"].ap, op=ALU.subtract), [S["rem"], S["pf"]], [S["rem"]])
            dve(lambda e: e.tensor_scalar(out=R32.ap, in0=Sel.ap, scalar1=S["pf"].ap[:, 0:1], scalar2=None, op0=ALU.mult), [Sel, S["pf"]], [R32])
            dve(lambda e, k=k: e.tensor_copy(out=Rb[k].ap, in_=R32.ap), [R32], [Rb[k]])
        for k in range(3):
            kb.op("pe", lambda e, k=k: e.matmul(psm.ap[:, 16:32], lhsT=onesb.ap, rhs=Rb[k].ap, start=(k == 0), stop=(k == 2)), reads=[onesb, Rb[k]], writes=[psm])
        dve(lambda e, gi=gi: e.tensor_copy(out=Tthr[gi].ap, in_=psm.ap[:, 16:32]), [psm], [Tthr[gi]])
    for (t0, t1, gi) in ((0, 16, 0), (16, NT, 1)):
        nt_ = t1 - t0
        dve(lambda e, t0=t0, t1=t1, gi=gi, nt_=nt_: e.tensor_tensor(out=GT.ap[:, t0:t1, :], in0=AL.ap[:, t0:t1, :],
                                                                in1=Tthr[gi].ap.unsqueeze(1).to_broadcast([128, nt_, NEXP]), op=ALU.is_ge), [AL, Tthr[gi]], [GT])
        dve(lambda e, t0=t0, t1=t1: e.tensor_tensor(out=GT.ap[:, t0:t1, :], in0=GT.ap[:, t0:t1, :], in1=AL.ap[:, t0:t1, :], op=ALU.mult), [GT, AL], [GT])

    stg = [kb.sb([128, 8, JB], F32, f"stg{i}") for i in range(2)]
    Wg = [kb.sb([128, 8, JB], BF16, f"Wg{i}") for i in range(2)]
    Wu = [kb.sb([128, 8, JB], BF16, f"Wu{i}") for i in range(2)]
    Wd = [kb.sb([128, JB // 128, D], BF16, f"Wd{i}") for i in range(2)]
    hT = kb.sb([128, 8, TG], BF16, "hT")
    sq = kb.sb([128, 8, JB], BF16, "sq")
    rb = kb.sb([128, JB], F32, "rb")
    hid = [kb.sb([128, JB // 128, 512], BF16, f"hid{i}") for i in range(2)]
    sg = [kb.sb([128, 512], F32, f"sg{i}") for i in range(2)]
    fin = [kb.sb([128, D], F32, f"fin{i}") for i in range(2)]
    junk = kb.sb([128, D], BF16, "junk")
    fs = [kb.sb([128, 2], F32, f"fs{i}") for i in range(2)]
    gps = [kb.ps([128, 512], F32, f"gps{i}") for i in range(2)]
    ups = [kb.ps([128, 512], F32, f"ups{i}") for i in range(2)]
    yps = [kb.ps([128, 512], F32, f"yps{i}") for i in range(2)]
    XA = big.ap[:, 0:(TG // 128) * D].rearrange("p (t d) -> p t d", d=D)
    xT_v = xmT.rearrange("(c p) t -> p c t", p=128)
    wg_v = wg.rearrange("e (c p) j -> e p c j", p=128)
    wu_v = wu.rearrange("e (c p) j -> e p c j", p=128)
    wd_v = wd.rearrange("e (jc p) n -> e p jc n", p=128)
    ctr = {"s": 0, "w": 0, "g": 0, "y": 0, "h": 0}
    for G in range(NG):
        tok0 = G * TG
        kb.dma("sp", XA, xmid[tok0:tok0 + TG, :].rearrange("(t p) d -> p t d", p=128), writes=[big])
        for q in range(TG // JB):
            st = stg[ctr["s"] % 2]; ctr["s"] += 1
            kb.dma("sp", st.ap, xT_v[:, :, tok0 + q * JB:tok0 + (q + 1) * JB], writes=[st])
            kb.op("act", lambda e, st=st: e.activation(out=sq.ap, in_=st.ap, func=AF.Square), reads=[st], writes=[sq])
            for c in range(8):
                kb.op("pe", lambda e, c=c: e.matmul(psm.ap[:, 0:JB], lhsT=onesb.ap, rhs=sq.ap[:, c, :], start=(c == 0), stop=(c == 7)), reads=[onesb, sq], writes=[psm])
            kb.op("act", lambda e: e.activation(out=rb.ap, in_=psm.ap[:, 0:JB], func=AF.Sqrt, scale=1.0 / D, bias=epsb.ap[:, 0:1]), reads=[psm, epsb], writes=[rb])
            kb.op("dve", lambda e: e.reciprocal(out=rb.ap, in_=rb.ap), reads=[rb], writes=[rb])
            for c in range(8):
                kb.op("dve", lambda e, c=c, st=st, q=q: e.scalar_tensor_tensor(out=hT.ap[:, c, q * JB:(q + 1) * JB], in0=st.ap[:, c, :], scalar=g2.ap[:, c:c + 1],
                                                                           in1=rb.ap, op0=ALU.mult, op1=ALU.mult), reads=[st, g2, rb], writes=[hT])
        for ex in range(NEXP):
            for jb in range(NJB):
                w = ctr["w"] % 2; ctr["w"] += 1
                for (src, dst) in ((wg_v[ex, :, :, jb * JB:(jb + 1) * JB], Wg[w]), (wu_v[ex, :, :, jb * JB:(jb + 1) * JB], Wu[w])):
                    st = stg[ctr["s"] % 2]; ctr["s"] += 1
                    kb.dma("sp", st.ap, src, writes=[st])
                    kb.op("pool", lambda e, st=st, dst=dst: e.tensor_copy(out=dst.ap, in_=st.ap), reads=[st], writes=[dst])
                st = stg[ctr["s"] % 2]; ctr["s"] += 1
                stv = st.ap.rearrange("p c j -> p (c j)").rearrange("p (jc n) -> p jc n", n=D)
                kb.dma("sp", stv, wd_v[ex, :, jb * (JB // 128):(jb + 1) * (JB // 128), :], writes=[st])
                kb.op("pool", lambda e, stv=stv, w=w: e.tensor_copy(out=Wd[w].ap, in_=stv), reads=[st], writes=[Wd[w]])
                for ck in range(TG // 512):
                    H = hid[ctr["h"] % 2]; ctr["h"] += 1
                    for jc in range(JB // 128):
                        gp = gps[ctr["g"] % 2]; up = ups[ctr["g"] % 2]; SG = sg[ctr["g"] % 2]; ctr["g"] += 1
                        for c in range(8):
                            kb.op("pe", lambda e, c=c, gp=gp, jc=jc: e.matmul(gp.ap, lhsT=Wg[w].ap[:, c, jc * 128:(jc + 1) * 128], rhs=hT.ap[:, c, ck * 512:(ck + 1) * 512],
                                                                         start=(c == 0), stop=(c == 7)), reads=[Wg[w], hT], writes=[gp])
                        for c in range(8):
                            kb.op("pe", lambda e, c=c, up=up, jc=jc: e.matmul(up.ap, lhsT=Wu[w].ap[:, c, jc * 128:(jc + 1) * 128], rhs=hT.ap[:, c, ck * 512:(ck + 1) * 512],
                                                                         start=(c == 0), stop=(c == 7)), reads=[Wu[w], hT], writes=[up])
                        kb.op("act", lambda e, gp=gp, SG=SG: e.activation(out=SG.ap, in_=gp.ap, func=AF.Silu), reads=[gp], writes=[SG])
                        kb.op("dve", lambda e, up=up, SG=SG, H=H, jc=jc: e.tensor_tensor(out=H.ap[:, jc, :], in0=up.ap, in1=SG.ap, op=ALU.mult), reads=[up, SG], writes=[H])
                    for tt in range(4):
                        tile = ck * 4 + tt
                        gtile = G * (TG // 128) + tile
                        for half in range(2):
                            yp = yps[ctr["y"] % 2]; ctr["y"] += 1
                            for jc in range(JB // 128):
                                kb.op("pe", lambda e, jc=jc, yp=yp, half=half, H=H, tt=tt: e.matmul(yp.ap, lhsT=H.ap[:, jc, tt * 128:(tt + 1) * 128], rhs=Wd[w].ap[:, jc, half * 512:(half + 1) * 512],
                                                                                          start=(jc == 0), stop=(jc == JB // 128 - 1)), reads=[H, Wd[w]], writes=[yp])
                            kb.op("dve", lambda e, yp=yp, half=half, tile=tile, gtile=gtile, ex=ex: e.scalar_tensor_tensor(
                                out=XA[:, tile, half * 512:(half + 1) * 512], in0=yp.ap, scalar=GT.ap[:, gtile, ex:ex + 1],
                                in1=XA[:, tile, half * 512:(half + 1) * 512], op0=ALU.mult, op1=ALU.add), reads=[yp, GT, big], writes=[big])
        for tile in range(TG // 128):
            r0 = tok0 + tile * 128
            if not final:
                kb.dma("pool", xo[r0:r0 + 128, :], XA[:, tile, :], reads=[big], is_output=True)
            else:
                F_, FS = fin[tile % 2], fs[tile % 2]
                kb.op("act", lambda e, tile=tile, FS=FS: e.activation(out=junk.ap, in_=XA[:, tile, :], func=AF.Square, accum_out=FS.ap[:, 0:1]), reads=[big], writes=[junk, FS])
                kb.op("act", lambda e, FS=FS: e.activation(out=FS.ap[:, 0:1], in_=FS.ap[:, 0:1], func=AF.Sqrt, scale=1.0 / D, bias=epsb.ap[:, 0:1]), reads=[FS, epsb], writes=[FS])
                kb.op("dve", lambda e, FS=FS: e.reciprocal(out=FS.ap[:, 0:1], in_=FS.ap[:, 0:1]), reads=[FS], writes=[FS])
                kb.op("dve", lambda e, tile=tile, F_=F_, FS=FS: e.scalar_tensor_tensor(out=F_.ap, in0=XA[:, tile, :], scalar=FS.ap[:, 0:1], in1=gf.ap, op0=ALU.mult, op1=ALU.mult),
                      reads=[big, FS, gf], writes=[F_])
                kb.dma("pool", xo[r0:r0 + 128, :], F_.ap, reads=[F_], is_output=True)
    return kb


def run_l3(xmids, affs, norm_ffn_l, wg_l, wu_l, wd_l, final_norm, final):
    kb = build_l3(final)
    affP = np.concatenate([a[:SEG] for a in affs], 0)
    affS = np.concatenate([a[SEG:] for a in affs], 0)

    def lay(a):
        n = a.shape[0]
        return np.ascontiguousarray(a.T.reshape(NEXP, 8, n // 8).reshape(128, n // 8))

    p = np.arange(128)
    BD = (p[:, None] // 8 == p[None, :] // 8).astype(np.float32)
    Sel = (p[:, None] == 8 * np.arange(NEXP)[None, :]).astype(np.float32)
    base = {"affP": lay(affP), "affS": lay(affS), "BD": BD, "Sel": Sel,
            "g2": np.ascontiguousarray(norm_ffn_l.reshape(8, 128).T),
            "gfin": np.ascontiguousarray(np.broadcast_to(final_norm, (128, D))),
            "wg": np.ascontiguousarray(wg_l), "wu": np.ascontiguousarray(wu_l), "wd": np.ascontiguousarray(wd_l)}
    maps = []
    for c in range(NCORES):
        m = dict(base)
        m["xmT"] = np.ascontiguousarray(xmids[c].T)
        m["xmid"] = np.ascontiguousarray(xmids[c])
        m["affl"] = np.ascontiguousarray(affs[c])
        maps.append(m)
    res = _run(kb, maps)
    return [r["xo"] for r in res]


def kernel(x_prompt, x_sample, w_in, w_out, norm_mix, norm_ffn, q_gain, k_gain, out_gain, na_rpb, rel_bias,
           w_router, w_gate, w_up, w_down, final_norm):
    f = lambda a: np.asarray(a, dtype=np.float32)
    xp, xs = f(x_prompt)[0], f(x_sample)
    w_in, w_out, norm_mix, norm_ffn, q_gain, k_gain, out_gain = map(f, (w_in, w_out, norm_mix, norm_ffn, q_gain, k_gain, out_gain))
    na_rpb, rel_bias, w_router, w_gate, w_up, w_down, final_norm = map(f, (na_rpb, rel_bias, w_router, w_gate, w_up, w_down, final_norm))
    xc = [core_tokens(xp, xs, c) for c in range(NCORES)]
    for l in range(DEPTH):
        projs = run_l1(xc, w_in[l], norm_mix[l], q_gain[l], k_gain[l])
        xm, aff = run_l2(projs, xc, w_out[l], out_gain[l], norm_ffn[l], w_router[l], rel_bias, na_rpb[l])
        xc = run_l3(xm, aff, norm_ffn[l], w_gate[l], w_up[l], w_down[l], final_norm, final=(l == DEPTH - 1))
    y_prompt = np.concatenate([xc[c][:SEG] for c in range(NCORES)], 0)[None]
    y_sample = np.stack([xc[s // 4][SEG * (1 + s % 4):SEG * (2 + s % 4)] for s in range(NSEQ_S)], 0)
    return (np.ascontiguousarray(y_prompt, dtype=np.float32), np.ascontiguousarray(y_sample, dtype=np.float32))
```

```python
import numpy as np
import ml_dtypes
import concourse.bass as bass
import concourse.mybir as mybir
from concourse.bass_utils import run_bass_kernel_spmd

F32 = mybir.dt.float32
BF16 = mybir.dt.bfloat16
I32 = mybir.dt.int32
ALU = mybir.AluOpType
AF = mybir.ActivationFunctionType
AX = mybir.AxisListType
NPBF = ml_dtypes.bfloat16

NCORES = 8
D = 1024
DEPTH = 2
SEQ_P = 16384
NSEQ_S = 32
SEQ_S = 2048
SEG = 2048
NSEG = 5
NTOK = NSEG * SEG
NT = NTOK // 128
HD = 64
D_IN = 3840
EPS = 1e-6
NEXP = 16
DEXP = 2048
NEG = -30000.0


class Buf:
    def __init__(self, ap):
        self.ap = ap
        self.w = None
        self.r = []

    def __getitem__(self, k):
        return self.ap[k]


class KB:
    R = 4
    NDMA = 24

    def __init__(self):
        self.nc = bass.Bass("TRN2", target_bir_lowering=False)
        nc = self.nc
        self.eng = {"pe": nc.tensor, "act": nc.scalar, "dve": nc.vector,
                    "pool": nc.gpsimd, "sp": nc.sync}
        self.sems = {e: [nc.alloc_semaphore(f"s_{e}{i}") for i in range(self.R)]
                     for e in ("pe", "act", "dve", "pool")}
        self.cnt = {e: 0 for e in ("pe", "act", "dve", "pool")}
        self.dsem = [nc.alloc_semaphore(f"s_dma{i}") for i in range(self.NDMA)]
        self.dval = [0] * self.NDMA
        self.dnext = 0
        self.waited = {}
        self.out_tokens = []
        self.nbuf = 0

    def sb(self, shape, dt, name=None):
        self.nbuf += 1
        return Buf(self.nc.alloc_sbuf_tensor(name or f"sb{self.nbuf}", list(shape), dt).ap())

    def ps(self, shape, dt=F32, name=None):
        self.nbuf += 1
        return Buf(self.nc.alloc_psum_tensor(name or f"ps{self.nbuf}", list(shape), dt).ap())

    def dram(self, name, shape, dt, kind):
        return self.nc.dram_tensor(name, list(shape), dt, kind=kind).ap()

    def _wait(self, waiter, tok):
        if tok is None:
            return
        key, n = tok
        if self.waited.get((waiter, key), 0) >= n:
            return
        self.waited[(waiter, key)] = n
        e = self.eng[waiter]
        if key[0] == "dma":
            e.wait_ge(self.dsem[key[1]], n * 16)
        else:
            e.wait_ge(self.sems[key[0]][(n - 1) % self.R], (n - 1) // self.R + 1)

    def _deps(self, waiter, reads, writes):
        for b in reads:
            self._wait(waiter, b.w)
        for b in writes:
            self._wait(waiter, b.w)
            for t in b.r:
                self._wait(waiter, t)

    def _commit(self, tok, reads, writes):
        for b in reads:
            b.r.append(tok)
            if len(b.r) > 64:
                b.r = b.r[-64:]
        for b in writes:
            b.w = tok
            b.r = []

    def op(self, e, fn, reads=(), writes=()):
        self._deps(e, reads, writes)
        ins = fn(self.eng[e])
        self.cnt[e] += 1
        n = self.cnt[e]
        ins.then_inc(self.sems[e][(n - 1) % self.R], 1)
        tok = ((e,), n)
        self._commit(tok, reads, writes)
        return tok

    def dma(self, q, out, in_, reads=(), writes=(), is_output=False, **kw):
        self._deps(q, reads, writes)
        i = self.dnext
        self.dnext = (self.dnext + 1) % self.NDMA
        if self.dval[i] > 0:
            self._wait(q, (("dma", i), self.dval[i]))
        self.dval[i] += 1
        self.eng[q].dma_start(out=out, in_=in_, **kw).then_inc(self.dsem[i], 16)
        tok = (("dma", i), self.dval[i])
        self._commit(tok, reads, writes)
        if is_output:
            self.out_tokens.append(tok)
        return tok

    def finish(self):
        for t in self.out_tokens:
            self._wait("sp", t)
        return self.nc


def _run(kb, in_maps):
    nc = kb.finish()
    res = run_bass_kernel_spmd(nc, in_maps, core_ids=list(range(NCORES)))
    return res.results


def build_l1():
    kb = KB()
    nc = kb.nc
    xT = kb.dram("xT", [D, NTOK], F32, "ExternalInput")
    w_in = kb.dram("w_in", [D, D_IN], F32, "ExternalInput")
    gmix = kb.dram("gmix", [128, 8], F32, "ExternalInput")
    gqk = kb.dram("gqk", [128, 640], F32, "ExternalInput")
    cosd = kb.dram("cos", [NTOK, 32], F32, "ExternalInput")
    sind = kb.dram("sin", [NTOK, 32], F32, "ExternalInput")
    proj = kb.dram("proj", [NTOK, D_IN], BF16, "ExternalOutput")

    gW = kb.sb([128, 8, D_IN], BF16, "gW")
    gm = kb.sb([128, 8], F32, "gm")
    gq = kb.sb([128, 640], F32, "gq")
    ones = kb.sb([128, 1], BF16, "ones")
    kb.dma("sp", gm.ap, gmix, writes=[gm])
    kb.dma("sp", gq.ap, gqk, writes=[gq])
    kb.op("pool", lambda e: e.memset(ones.ap, 1.0), writes=[ones])
    epsb = kb.sb([128, 1], F32, "epsb")
    kb.op("pool", lambda e: e.memset(epsb.ap, EPS), writes=[epsb])
    wst = [kb.sb([128, 1920], F32, f"wst{i}") for i in range(2)]
    w_v = w_in.rearrange("(c p) n -> p c n", p=128)
    k = 0
    for c in range(8):
        for hf in range(2):
            st = wst[k % 2]
            k += 1
            kb.dma("sp", st.ap, w_v[:, c, hf * 1920:(hf + 1) * 1920], writes=[st])
            kb.op("dve", lambda e, st=st, c=c, hf=hf: e.tensor_scalar(
                out=gW.ap[:, c, hf * 1920:(hf + 1) * 1920], in0=st.ap, scalar1=gm.ap[:, c:c + 1],
                scalar2=None, op0=ALU.mult), reads=[st, gm], writes=[gW])

    NB = 2
    xs = [kb.sb([128, 8, 128], F32, f"xs{i}") for i in range(NB)]
    xb = [kb.sb([128, 8, 128], BF16, f"xb{i}") for i in range(NB)]
    sq = [kb.sb([128, 8, 128], BF16, f"sq{i}") for i in range(NB)]
    cs = [kb.sb([128, 32], F32, f"cs{i}") for i in range(NB)]
    sn = [kb.sb([128, 32], F32, f"sn{i}") for i in range(NB)]
    rstd = [kb.sb([128, 1], F32, f"rstd{i}") for i in range(NB)]
    ot = [kb.sb([128, 640], F32, f"ot{i}") for i in range(NB)]
    ob = [kb.sb([128, D_IN], BF16, f"ob{i}") for i in range(NB)]
    rs8 = [kb.sb([128, 1], F32, f"rs8{i}") for i in range(NB)]
    tA = [kb.sb([128, 640], F32, f"tA{i}") for i in range(NB)]
    tB = [kb.sb([128, 640], F32, f"tB{i}") for i in range(NB)]
    st10 = [kb.sb([128, 10], F32, f"st10{i}") for i in range(NB)]
    pss = kb.ps([128, 1], F32, "pss")
    psp = [kb.ps([128, 512], F32, f"psp{i}") for i in range(4)]
    xT_v = xT.rearrange("(c p) t -> p c t", p=128)
    colchunks = [(i * 512, min(512, D_IN - i * 512)) for i in range(8)]
    pk = 0
    CATS = [(0, 640, "a"), (640, 768, "p"), (768, 1536, "q"), (1536, 3072, "p"), (3072, 3328, "q"), (3328, 3840, "p")]
    for t in range(NT):
        b = t % NB
        X, XB, SQ, CS, SN, RS, OT, TA, TB, S10 = xs[b], xb[b], sq[b], cs[b], sn[b], rstd[b], ot[b], tA[b], tB[b], st10[b]
        OB, RS8 = ob[b], rs8[b]
        kb.dma("sp", X.ap, xT_v[:, :, t * 128:(t + 1) * 128], writes=[X])
        kb.dma("sp", CS.ap, cosd[t * 128:(t + 1) * 128, :], writes=[CS])
        kb.dma("sp", SN.ap, sind[t * 128:(t + 1) * 128, :], writes=[SN])
        kb.op("dve", lambda e: e.tensor_copy(out=XB.ap, in_=X.ap), reads=[X], writes=[XB])
        kb.op("act", lambda e: e.activation(out=SQ.ap, in_=X.ap, func=AF.Square), reads=[X], writes=[SQ])
        for c in range(8):
            kb.op("pe", lambda e, c=c: e.matmul(pss.ap, lhsT=SQ.ap[:, c, :], rhs=ones.ap, start=(c == 0), stop=(c == 7)),
                  reads=[SQ, ones], writes=[pss])
        kb.op("act", lambda e: e.activation(out=RS.ap, in_=pss.ap, func=AF.Sqrt, scale=1.0 / D, bias=epsb.ap[:, 0:1]),
              reads=[pss, epsb], writes=[RS])
        kb.op("dve", lambda e: e.reciprocal(out=RS.ap, in_=RS.ap), reads=[RS], writes=[RS])
        kb.op("dve", lambda e: e.tensor_scalar(out=RS8.ap, in0=RS.ap, scalar1=0.125, scalar2=None, op0=ALU.mult), reads=[RS], writes=[RS8])
        for (c0, w) in colchunks:
            P = psp[pk % 4]
            pk += 1
            for c in range(8):
                kb.op("pe", lambda e, c=c, P=P, c0=c0, w=w: e.matmul(P.ap[:, 0:w], lhsT=XB.ap[:, c, :], rhs=gW.ap[:, c, c0:c0 + w],
                                                               start=(c == 0), stop=(c == 7)), reads=[XB, gW], writes=[P])
            for (a0, a1, cat) in CATS:
                lo, hi = max(a0, c0), min(a1, c0 + w)
                if lo >= hi:
                    continue
                if cat == "a":
                    kb.op("act", lambda e, P=P, lo=lo, hi=hi, c0=c0: e.activation(out=OT.ap[:, lo:hi], in_=P.ap[:, lo - c0:hi - c0], func=AF.Copy, scale=RS.ap[:, 0:1]),
                          reads=[P, RS], writes=[OT])
                else:
                    SC = RS8 if cat == "q" else RS
                    kb.op("act", lambda e, P=P, lo=lo, hi=hi, c0=c0, SC=SC: e.activation(out=OB.ap[:, lo:hi], in_=P.ap[:, lo - c0:hi - c0], func=AF.Copy, scale=SC.ap[:, 0:1]),
                          reads=[P, SC], writes=[OB])
        y3 = OT.ap.rearrange("p (h d) -> p h d", d=64)
        ta3 = TA.ap.rearrange("p (h d) -> p h d", d=64)
        tb3 = TB.ap.rearrange("p (h d) -> p h d", d=64)
        kb.op("dve", lambda e: e.tensor_tensor(out=TA.ap, in0=OT.ap, in1=OT.ap, op=ALU.mult), reads=[OT], writes=[TA])
        kb.op("dve", lambda e: e.tensor_reduce(out=S10.ap, in_=ta3, axis=AX.X, op=ALU.add), reads=[TA], writes=[S10])
        kb.op("act", lambda e: e.activation(out=S10.ap, in_=S10.ap, func=AF.Sqrt, scale=1.0 / HD, bias=epsb.ap[:, 0:1]), reads=[S10, epsb], writes=[S10])
        kb.op("dve", lambda e: e.reciprocal(out=S10.ap, in_=S10.ap), reads=[S10], writes=[S10])
        kb.op("dve", lambda e: e.tensor_scalar(out=S10.ap[:, 0:8], in0=S10.ap[:, 0:8], scalar1=0.125, scalar2=None, op0=ALU.mult), reads=[S10], writes=[S10])
        kb.op("dve", lambda e: e.tensor_tensor(out=ta3, in0=y3, in1=S10.ap.unsqueeze(2).to_broadcast([128, 10, 64]), op=ALU.mult), reads=[OT, S10], writes=[TA])
        kb.op("dve", lambda e: e.tensor_tensor(out=TA.ap, in0=TA.ap, in1=gq.ap, op=ALU.mult), reads=[TA, gq], writes=[TA])
        x4 = TA.ap.rearrange("p (h i two) -> p h i two", i=32, two=2)
        o4 = OB.ap[:, 0:640].rearrange("p (h i two) -> p h i two", i=32, two=2)
        t4 = TB.ap.rearrange("p (h i two) -> p h i two", i=32, two=2)
        cb = CS.ap.unsqueeze(1).to_broadcast([128, 10, 32])
        sb_ = SN.ap.unsqueeze(1).to_broadcast([128, 10, 32])
        x1, x2 = x4[:, :, :, 0], x4[:, :, :, 1]
        kb.op("dve", lambda e: e.tensor_tensor(out=t4[:, :, :, 0], in0=x1, in1=cb, op=ALU.mult), reads=[TA, CS], writes=[TB])
        kb.op("dve", lambda e: e.tensor_tensor(out=t4[:, :, :, 1], in0=x2, in1=sb_, op=ALU.mult), reads=[TA, SN], writes=[TB])
        kb.op("dve", lambda e: e.tensor_tensor(out=o4[:, :, :, 0], in0=t4[:, :, :, 0], in1=t4[:, :, :, 1], op=ALU.subtract), reads=[TB], writes=[OB])
        kb.op("dve", lambda e: e.tensor_tensor(out=t4[:, :, :, 0], in0=x1, in1=sb_, op=ALU.mult), reads=[TA, SN], writes=[TB])
        kb.op("dve", lambda e: e.tensor_tensor(out=t4[:, :, :, 1], in0=x2, in1=cb, op=ALU.mult), reads=[TA, CS], writes=[TB])
        kb.op("dve", lambda e: e.tensor_tensor(out=o4[:, :, :, 1], in0=t4[:, :, :, 0], in1=t4[:, :, :, 1], op=ALU.add), reads=[TB], writes=[OB])
        kb.dma("pool", proj[t * 128:(t + 1) * 128, :], OB.ap, reads=[OB], is_output=True)
    return kb


def rope_tables(pos):
    row = (pos // 64).astype(np.float32)
    col = (pos % 64).astype(np.float32)
    half = HD // 2
    freqs = (np.float32(10000.0) ** (-np.arange(0, half, 2, dtype=np.float32) / np.float32(half))).astype(np.float32)
    ang = np.concatenate([row[:, None] * freqs, col[:, None] * freqs], axis=-1).astype(np.float32)
    return np.cos(ang).astype(np.float32), np.sin(ang).astype(np.float32)


def core_tokens(xp, xs, c):
    return np.concatenate([xp[c * SEG:(c + 1) * SEG]] + [xs[4 * c + i] for i in range(4)], axis=0)


def run_l1(xcores, w_in_l, norm_mix_l, q_gain_l, k_gain_l):
    kb = build_l1()
    gq = np.concatenate([np.tile(q_gain_l, 8), np.tile(k_gain_l, 2)]).astype(np.float32)
    maps = []
    for c in range(NCORES):
        pos = np.concatenate([np.arange(c * SEG, (c + 1) * SEG)] + [np.arange(SEG)] * 4)
        cs, sn = rope_tables(pos)
        maps.append({
            "xT": np.ascontiguousarray(xcores[c].T),
            "w_in": np.ascontiguousarray(w_in_l),
            "gmix": np.ascontiguousarray(norm_mix_l.reshape(8, 128).T),
            "gqk": np.ascontiguousarray(np.broadcast_to(gq, (128, 640))),
            "cos": cs, "sin": sn,
        })
    res = _run(kb, maps)
    return [r["proj"] for r in res]


PADSEG = 4096
HALO = 1024
B_GR = [(1, 64), (4, 256), (16, 1024)]
B_NT = [5, 8, 20]
B_SW = [128 * (n - 1) + 512 for n in B_NT]
B_SO = [0, B_SW[0], B_SW[0] + B_SW[1]]
B_STRIP = sum(B_SW)
C_NT = 8
NQB = NTOK // 512


def build_l2():
    kb = KB()
    QT = kb.dram("QT", [24, 64, NTOK], BF16, "ExternalInput")
    KTAp = kb.dram("KTAp", [2, 64, SEQ_P], BF16, "ExternalInput")
    VAp = kb.dram("VAp", [2, SEQ_P, 65], BF16, "ExternalInput")
    KTAs = kb.dram("KTAs", [2, 64, 4 * SEG], BF16, "ExternalInput")
    VAs = kb.dram("VAs", [2, 4 * SEG, 65], BF16, "ExternalInput")
    KTB = kb.dram("KTB", [12, 64, NSEG * PADSEG], BF16, "ExternalInput")
    VB = kb.dram("VB", [12, NSEG * PADSEG, 65], BF16, "ExternalInput")
    KTC = kb.dram("KTC", [4, 64, NSEG * PADSEG], BF16, "ExternalInput")
    VC = kb.dram("VC", [4, NSEG * PADSEG, 65], BF16, "ExternalInput")
    stripB = kb.dram("stripB", [128, 4, B_STRIP], F32, "ExternalInput")
    biasC = kb.dram("biasC", [5, 4, C_NT, 128, 512], F32, "ExternalInput")
    xin = kb.dram("x", [NTOK, D], F32, "ExternalInput")
    w_out = kb.dram("w_out", [D, D], F32, "ExternalInput")
    gout = kb.dram("gout", [64, 16], F32, "ExternalInput")
    g2d = kb.dram("g2", [128, 8], F32, "ExternalInput")
    w_r = kb.dram("w_r", [D, NEXP], F32, "ExternalInput")
    xmid = kb.dram("xmid", [NTOK, D], F32, "ExternalOutput")
    affo = kb.dram("aff", [NTOK, NEXP], F32, "ExternalOutput")

    stage = kb.sb([128, 4096], F32, "stage")
    sB = kb.sb([128, 4, B_STRIP], BF16, "sB")
    gWo = kb.sb([64, 16, D], BF16, "gWo")
    go = kb.sb([64, 16], F32, "go")
    g2 = kb.sb([128, 8], F32, "g2sb")
    identb = kb.sb([128, 128], BF16, "identb")
    identf = kb.sb([128, 128], F32, "identf")
    onesb = kb.sb([128, 64], BF16, "onesb")
    epsb = kb.sb([128, 1], F32, "epsb")
    gw32 = kb.sb([128, 8, NEXP], F32, "gw32")
    gwh = kb.sb([128, 8, NEXP], BF16, "gwh")
    gwl = kb.sb([128, 8, NEXP], BF16, "gwl")
    kb.op("pool", lambda e: e.memset(identf.ap, 0.0), writes=[identf])
    kb.op("pool", lambda e: e.affine_select(out=identf.ap, in_=identf.ap, pattern=[[-1, 128]], compare_op=ALU.not_equal,
                                            fill=1.0, base=0, channel_multiplier=1), reads=[identf], writes=[identf])
    kb.op("dve", lambda e: e.tensor_copy(out=identb.ap, in_=identf.ap), reads=[identf], writes=[identb])
    kb.op("pool", lambda e: e.memset(onesb.ap, 1.0), writes=[onesb])
    kb.op("pool", lambda e: e.memset(epsb.ap, EPS), writes=[epsb])
    kb.dma("sp", go.ap, gout, writes=[go])
    kb.dma("sp", g2.ap, g2d, writes=[g2])
    for h in range(4):
        for (o, w) in ((0, 4096), (4096, B_STRIP - 4096)):
            kb.dma("sp", stage.ap[:, 0:w], stripB[:, h, o:o + w], writes=[stage])
            kb.op("dve", lambda e, h=h, o=o, w=w: e.tensor_copy(out=sB.ap[:, h, o:o + w], in_=stage.ap[:, 0:w]), reads=[stage], writes=[sB])
    wo_v = w_out.rearrange("(s d) n -> d s n", d=64)
    for s4 in range(4):
        stv = stage.ap[0:64, :].rearrange("p (s n) -> p s n", s=4)
        kb.dma("sp", stv, wo_v[:, s4 * 4:(s4 + 1) * 4, :], writes=[stage])
        for s in range(4):
            sl = s4 * 4 + s
            kb.op("dve", lambda e, s=s, sl=sl, stv=stv: e.tensor_scalar(out=gWo.ap[:, sl, :], in0=stv[:, s, :], scalar1=go.ap[:, sl:sl + 1],
                                                                        scalar2=None, op0=ALU.mult), reads=[stage, go], writes=[gWo])
    kb.dma("sp", gw32.ap, w_r.rearrange("(c p) e -> p c e", p=128), writes=[gw32])
    kb.op("dve", lambda e: e.tensor_tensor(out=gw32.ap, in0=gw32.ap, in1=g2.ap.unsqueeze(2).to_broadcast([128, 8, NEXP]), op=ALU.mult),
          reads=[gw32, g2], writes=[gw32])
    kb.op("dve", lambda e: e.tensor_copy(out=gwh.ap, in_=gw32.ap), reads=[gw32], writes=[gwh])
    kb.op("dve", lambda e: e.tensor_tensor(out=gwl.ap, in0=gw32.ap, in1=gwh.ap, op=ALU.subtract), reads=[gw32, gwh], writes=[gwl])

    qbuf = [kb.sb([64, 4, 512], BF16, f"qbuf{i}") for i in range(2)]
    kbuf = [kb.sb([64, 2560], BF16, f"kbuf{i}") for i in range(2)]
    vbuf = [kb.sb([128, 20, 65], BF16, f"vbuf{i}") for i in range(2)]
    bcb = [kb.sb([128, C_NT, 512], BF16, f"bcb{i}") for i in range(2)]
    PT = [kb.sb([128, 512], BF16, f"PT{i}") for i in range(3)]
    mixT = kb.sb([64, 16, 512], BF16, "mixT")
    osq = kb.sb([64, 16, 128], BF16, "osq")
    rden = kb.sb([128, 512], F32, "rden")
    rh = kb.sb([128, 512], BF16, "rh")
    rl = kb.sb([128, 512], BF16, "rl")
    bcs = kb.sb([64, 512], F32, "bcs")
    xm = [kb.sb([128, D], F32, f"xm{i}") for i in range(2)]
    xh = kb.sb([128, 8, 128], BF16, "xh")
    xl = kb.sb([128, 8, 128], BF16, "xl")
    junk = kb.sb([128, D], BF16, "junk")
    sm = [kb.sb([128, 8], F32, f"sm{i}") for i in range(2)]
    lg = [kb.sb([128, NEXP], F32, f"lg{i}") for i in range(2)]
    acc = [kb.ps([128, 512], F32, f"acc{i}") for i in range(4)]
    stp = [kb.ps([128, 512], F32, f"stp{i}") for i in range(2)]
    bcp = kb.ps([128, 512], F32, "bcp")
    cnt = {"k": 0, "q": 0, "s": 0, "p": 0, "c": 0}

    def nxt(name, lst):
        b = lst[cnt[name] % len(lst)]
        cnt[name] += 1
        return b

    def score_pv(K, koff, Q, qs, V, vt, A, first, last, bias=None):
        S = nxt("s", stp)
        P = nxt("p", PT)
        kb.op("pe", lambda e: e.matmul(S.ap, lhsT=K.ap[:, koff:koff + 128], rhs=Q.ap[:, qs, :], start=True, stop=(bias is None)),
              reads=[K, Q], writes=[S])
        if bias is not None:
            bbuf, bap = bias
            kb.op("pe", lambda e: e.matmul(S.ap, lhsT=identb.ap, rhs=bap, start=False, stop=True), reads=[identb, bbuf], writes=[S])
        kb.op("act", lambda e: e.activation(out=P.ap, in_=S.ap, func=AF.Exp), reads=[S], writes=[P])
        kb.op("pe", lambda e: e.matmul(A.ap[0:65, :], lhsT=V.ap[:, vt, :], rhs=P.ap, start=first, stop=last), reads=[V, P], writes=[A])

    def finalize(A, slot):
        kb.op("dve", lambda e: e.reciprocal(out=rden.ap[64:65, :], in_=A.ap[64:65, :]), reads=[A], writes=[rden])
        kb.op("dve", lambda e: e.tensor_copy(out=rh.ap[64:65, :], in_=rden.ap[64:65, :]), reads=[rden], writes=[rh])
        kb.op("dve", lambda e: e.tensor_tensor(out=rl.ap[64:65, :], in0=rden.ap[64:65, :], in1=rh.ap[64:65, :], op=ALU.subtract),
              reads=[rden, rh], writes=[rl])
        kb.op("pe", lambda e: e.matmul(bcp.ap[0:64, :], lhsT=onesb.ap[64:65, 0:64], rhs=rh.ap[64:65, :], start=True, stop=False),
              reads=[onesb, rh], writes=[bcp])
        kb.op("pe", lambda e: e.matmul(bcp.ap[0:64, :], lhsT=onesb.ap[64:65, 0:64], rhs=rl.ap[64:65, :], start=False, stop=True),
              reads=[onesb, rl], writes=[bcp])
        kb.op("act", lambda e: e.activation(out=bcs.ap, in_=bcp.ap[0:64, :], func=AF.Copy), reads=[bcp], writes=[bcs])
        kb.op("dve", lambda e: e.tensor_tensor(out=mixT.ap[:, slot, :], in0=A.ap[0:64, :], in1=bcs.ap, op=ALU.mult), reads=[A, bcs], writes=[mixT])

    def load_kv(KTd, Vd, col0, ntile):
        K = nxt("k", kbuf)
        V = vbuf[(cnt["k"] - 1) % 2]
        kb.dma("sp", K.ap[:, 0:ntile * 128], KTd[:, col0:col0 + ntile * 128], writes=[K])
        kb.dma("sp", V.ap[:, 0:ntile, :], Vd[col0:col0 + ntile * 128, :].rearrange("(t p) c -> p t c", p=128), writes=[V])
        return K, V

    for qb in range(NQB):
        seg, qbl = qb // 4, qb % 4
        t0 = qb * 512
        for j in range(2):
            Q = nxt("q", qbuf)
            kb.dma("sp", Q.ap, QT[4 * j:4 * j + 4, :, t0:t0 + 512].rearrange("s d t -> d s t"), writes=[Q])
            if seg == 0:
                chunks = [(KTAp[j], VAp[j], kc * 2048) for kc in range(8)]
            else:
                chunks = [(KTAs[j], VAs[j], (seg - 1) * SEG)]
            for ci, (KTd, Vd, col0) in enumerate(chunks):
                K, V = load_kv(KTd, Vd, col0, 16)
                for kt in range(16):
                    first = (ci == 0 and kt == 0)
                    last = (ci == len(chunks) - 1 and kt == 15)
                    for qh in range(4):
                        score_pv(K, kt * 128, Q, qh, V, kt, acc[qh], first, last)
            for qh in range(4):
                finalize(acc[qh], 4 * j + qh)
        for h in range(4):
            Q = nxt("q", qbuf)
            kb.dma("sp", Q.ap[:, 0:3, :], QT[8 + h:20:4, :, t0:t0 + 512].rearrange("s d t -> d s t"), writes=[Q])
            A = acc[h]
            for g in range(3):
                reach, nt = B_GR[g][1], B_NT[g]
                col0 = seg * PADSEG + HALO + 512 * qbl - reach
                K, V = load_kv(KTB[g * 4 + h], VB[g * 4 + h], col0, nt)
                for kt in range(nt):
                    so = B_SO[g] + 128 * (nt - 1 - kt)
                    score_pv(K, kt * 128, Q, g, V, kt, A, (g == 0 and kt == 0), (g == 2 and kt == nt - 1),
                             bias=(sB, sB.ap[:, h, so:so + 512]))
            finalize(A, 8 + h)
        if seg == 0:
            cls = {0: 3, 3: 4}.get(qbl, 1)
        else:
            cls = {0: 0, 3: 2}.get(qbl, 1)
        for h in range(4):
            Q = nxt("q", qbuf)
            kb.dma("sp", Q.ap[:, 0:1, :], QT[20 + h:21 + h, :, t0:t0 + 512].rearrange("s d t -> d s t"), writes=[Q])
            A = acc[h]
            col0 = seg * PADSEG + HALO + 512 * qbl - 256
            K, V = load_kv(KTC[h], VC[h], col0, C_NT)
            BC = nxt("c", bcb)
            stv = stage.ap.rearrange("p (k n) -> p k n", k=C_NT)
            kb.dma("sp", stv, biasC[cls, h].rearrange("k p n -> p k n"), writes=[stage])
            kb.op("dve", lambda e, BC=BC, stv=stv: e.tensor_copy(out=BC.ap, in_=stv), reads=[stage], writes=[BC])
            for kt in range(C_NT):
                score_pv(K, kt * 128, Q, 0, V, kt, A, kt == 0, kt == C_NT - 1, bias=(BC, BC.ap[:, kt, :]))
            finalize(A, 12 + h)
        for tt in range(4):
            tok0 = t0 + tt * 128
            X = xm[tt % 2]
            SM = sm[tt % 2]
            LG = lg[tt % 2]
            kb.dma("sp", X.ap, xin[tok0:tok0 + 128, :], writes=[X])
            kb.op("dve", lambda e: e.tensor_tensor(out=osq.ap, in0=mixT.ap[:, :, tt * 128:(tt + 1) * 128], in1=mixT.ap[:, :, tt * 128:(tt + 1) * 128], op=ALU.mult),
                  reads=[mixT], writes=[osq])
            for m, (s0, s1, width) in enumerate(((0, 8, 512), (8, 12, 256), (12, 16, 256))):
                for s in range(s0, s1):
                    kb.op("pe", lambda e, s=s: e.matmul(acc[2].ap[:, 0:1], lhsT=osq.ap[:, s, :], rhs=onesb.ap[0:64, 0:1], start=(s == s0), stop=(s == s1 - 1)),
                          reads=[osq, onesb], writes=[acc[2]])
                kb.op("act", lambda e, m=m, width=width: e.activation(out=SM.ap[:, m:m + 1], in_=acc[2].ap[:, 0:1], func=AF.Sqrt, scale=1.0 / width, bias=epsb.ap[:, 0:1]),
                      reads=[acc[2], epsb], writes=[SM])
                kb.op("dve", lambda e, m=m: e.reciprocal(out=SM.ap[:, m:m + 1], in_=SM.ap[:, m:m + 1]), reads=[SM], writes=[SM])
                for half in range(2):
                    O = acc[half]
                    for s in range(s0, s1):
                        kb.op("pe", lambda e, s=s, O=O, half=half: e.matmul(O.ap, lhsT=mixT.ap[:, s, tt * 128:(tt + 1) * 128], rhs=gWo.ap[:, s, half * 512:(half + 1) * 512],
                                                                       start=(s == s0), stop=(s == s1 - 1)), reads=[mixT, gWo], writes=[O])
                    kb.op("dve", lambda e, O=O, half=half, m=m: e.scalar_tensor_tensor(out=X.ap[:, half * 512:(half + 1) * 512], in0=O.ap, scalar=SM.ap[:, m:m + 1],
                                                                                     in1=X.ap[:, half * 512:(half + 1) * 512], op0=ALU.mult, op1=ALU.add),
                          reads=[O, SM, X], writes=[X])
            kb.dma("pool", xmid[tok0:tok0 + 128, :], X.ap, reads=[X], is_output=True)
            kb.op("act", lambda e: e.activation(out=junk.ap, in_=X.ap, func=AF.Square, accum_out=SM.ap[:, 3:4]), reads=[X], writes=[junk, SM])
            kb.op("act", lambda e: e.activation(out=SM.ap[:, 3:4], in_=SM.ap[:, 3:4], func=AF.Sqrt, scale=1.0 / D, bias=epsb.ap[:, 0:1]), reads=[SM, epsb], writes=[SM])
            kb.op("dve", lambda e: e.reciprocal(out=SM.ap[:, 3:4], in_=SM.ap[:, 3:4]), reads=[SM], writes=[SM])
            for c in range(8):
                kb.op("pe", lambda e, c=c: e.transpose(acc[3].ap[:, (c % 4) * 128:(c % 4 + 1) * 128], X.ap[:, c * 128:(c + 1) * 128], identf.ap),
                      reads=[X, identf], writes=[acc[3]])
                kb.op("act", lambda e, c=c: e.activation(out=xh.ap[:, c, :], in_=acc[3].ap[:, (c % 4) * 128:(c % 4 + 1) * 128], func=AF.Copy), reads=[acc[3]], writes=[xh])
                kb.op("dve", lambda e, c=c: e.tensor_tensor(out=xl.ap[:, c, :], in0=acc[3].ap[:, (c % 4) * 128:(c % 4 + 1) * 128], in1=xh.ap[:, c, :], op=ALU.subtract),
                      reads=[acc[3], xh], writes=[xl])
            k = 0
            for c in range(8):
                for (xa, wa) in ((xh, gwh), (xh, gwl), (xl, gwh)):
                    kb.op("pe", lambda e, c=c, xa=xa, wa=wa, k=k: e.matmul(acc[2].ap[:, 16:32], lhsT=xa.ap[:, c, :], rhs=wa.ap[:, c, :], start=(k == 0), stop=(k == 23)),
                          reads=[xa, wa], writes=[acc[2]])
                    k += 1
            kb.op("act", lambda e: e.activation(out=LG.ap, in_=acc[2].ap[:, 16:32], func=AF.Copy, scale=SM.ap[:, 3:4]), reads=[acc[2], SM], writes=[LG])
            kb.op("dve", lambda e: e.tensor_reduce(out=SM.ap[:, 4:5], in_=LG.ap, axis=AX.X, op=ALU.max), reads=[LG], writes=[SM])
            kb.op("dve", lambda e: e.tensor_scalar(out=SM.ap[:, 4:5], in0=SM.ap[:, 4:5], scalar1=-1.0, scalar2=None, op0=ALU.mult), reads=[SM], writes=[SM])
            kb.op("act", lambda e: e.activation(out=LG.ap, in_=LG.ap, func=AF.Exp, bias=SM.ap[:, 4:5], accum_out=SM.ap[:, 5:6]), reads=[LG, SM], writes=[LG, SM])
            kb.op("dve", lambda e: e.reciprocal(out=SM.ap[:, 5:6], in_=SM.ap[:, 5:6]), reads=[SM], writes=[SM])
            kb.op("dve", lambda e: e.tensor_scalar(out=LG.ap, in0=LG.ap, scalar1=SM.ap[:, 5:6], scalar2=None, op0=ALU.mult), reads=[LG, SM], writes=[LG])
            kb.dma("pool", affo[tok0:tok0 + 128, :], LG.ap, reads=[LG], is_output=True)
    return kb


def _t5_bucket(rel):
    nb, max_exact = 16, 8
    ret = (rel > 0).astype(np.int32) * nb
    n = np.abs(rel)
    lg = np.log(np.maximum(n, 1).astype(np.float32) / np.float32(max_exact)) / np.float32(np.log(2048.0 / max_exact)) * np.float32(nb - max_exact)
    large = np.minimum(max_exact + lg.astype(np.float32).astype(np.int32), nb - 1)
    return ret + np.where(n < max_exact, n, large)


def _strip_b(rel_bias):
    out = np.full((128, 4, B_STRIP), NEG, np.float32)
    i = np.arange(128)[:, None]
    for g, (d, reach) in enumerate(B_GR):
        nt, w = B_NT[g], B_SW[g]
        m = np.arange(w)[None, :]
        rel = i - m + 128 * (nt - 1) - reach
        valid = (rel % d == 0) & (np.abs(rel) <= 64 * d)
        bk = _t5_bucket(rel)
        for h in range(4):
            vals = rel_bias[bk, g * 4 + h]
            out[:, h, B_SO[g]:B_SO[g] + w] = np.where(valid, vals, np.float32(NEG))
    return out


def _bias_c(rpb, R, rows):
    out = np.full((4, C_NT, 128, 512), NEG, np.float32)
    r = (8 * R + np.arange(8))[:, None].repeat(64, 1).reshape(-1)
    c = np.tile(np.arange(64), 8)
    rs = np.clip(r - 4, 0, rows - 8)
    cs = np.clip(c - 8, 0, 64 - 16)
    for kt in range(C_NT):
        kr = (8 * R - 4 + 2 * kt + np.arange(2))[:, None].repeat(64, 1).reshape(-1)
        ck = np.tile(np.arange(64), 2)
        valid = ((kr[:, None] >= rs[None, :]) & (kr[:, None] <= rs[None, :] + 7) & (kr[:, None] >= 0) & (kr[:, None] < rows)
                 & (ck[:, None] >= cs[None, :]) & (ck[:, None] < cs[None, :] + 16))
        dr = np.clip(kr[:, None] - r[None, :] + 7, 0, 14)
        dc = np.clip(ck[:, None] - c[None, :], -15, 15) + 15
        for h in range(4):
            out[h, kt] = np.where(valid, rpb[h][dr, dc], np.float32(NEG))
    return out


def _aug(v, valid):
    return np.concatenate([v, valid[:, None].astype(v.dtype)], axis=1)


def run_l2(projs, xcores, w_out_l, out_gain_l, norm_ffn_l, w_r_l, rel_bias, na_rpb_l):
    kb = build_l2()
    Pp = np.concatenate([projs[c][:SEG] for c in range(NCORES)], 0)
    one_p = np.ones(SEQ_P, bool)
    KTAp = np.ascontiguousarray(np.stack([Pp[:, 512 + 64 * j:576 + 64 * j].T for j in range(2)]))
    VAp = np.ascontiguousarray(np.stack([_aug(Pp[:, 640 + 64 * j:704 + 64 * j], one_p) for j in range(2)]))
    stripB = _strip_b(rel_bias)
    bc_first, bc_mid, bc_last = _bias_c(na_rpb_l, 0, 32), _bias_c(na_rpb_l, 1, 32), _bias_c(na_rpb_l, 3, 32)
    qcols = [64 * s for s in range(8)] + [768 + 64 * k for k in range(12)] + [3072 + 64 * h for h in range(4)]
    maps = []
    for c in range(NCORES):
        loc = projs[c]
        QT = np.ascontiguousarray(np.stack([loc[:, q:q + 64].T for q in qcols]))
        ls = loc[SEG:]
        one_s = np.ones(4 * SEG, bool)
        KTAs = np.ascontiguousarray(np.stack([ls[:, 512 + 64 * j:576 + 64 * j].T for j in range(2)]))
        VAs = np.ascontiguousarray(np.stack([_aug(ls[:, 640 + 64 * j:704 + 64 * j], one_s) for j in range(2)]))
        pad = np.zeros((NSEG * PADSEG, D_IN), loc.dtype)
        val = np.zeros(NSEG * PADSEG, bool)
        g0, g1 = max(0, SEG * c - HALO), min(SEQ_P, SEG * c + SEG + HALO)
        o0 = g0 - (SEG * c - HALO)
        pad[o0:o0 + (g1 - g0)] = Pp[g0:g1]
        val[o0:o0 + (g1 - g0)] = True
        for s in range(1, NSEG):
            pad[s * PADSEG + HALO:s * PADSEG + HALO + SEG] = loc[s * SEG:(s + 1) * SEG]
            val[s * PADSEG + HALO:s * PADSEG + HALO + SEG] = True
        KTB = np.ascontiguousarray(np.stack([pad[:, 1536 + 64 * k:1600 + 64 * k].T for k in range(12)]))
        VB = np.ascontiguousarray(np.stack([_aug(pad[:, 2304 + 64 * k:2368 + 64 * k], val) for k in range(12)]))
        KTC = np.ascontiguousarray(np.stack([pad[:, 3328 + 64 * h:3392 + 64 * h].T for h in range(4)]))
        VC = np.ascontiguousarray(np.stack([_aug(pad[:, 3584 + 64 * h:3648 + 64 * h], val) for h in range(4)]))
        biasC = np.stack([bc_first, bc_mid, bc_last, _bias_c(na_rpb_l, 4 * c, 256), _bias_c(na_rpb_l, 4 * c + 3, 256)])
        maps.append({
            "QT": QT, "KTAp": KTAp, "VAp": VAp, "KTAs": KTAs, "VAs": VAs, "KTB": KTB, "VB": VB, "KTC": KTC, "VC": VC,
            "stripB": stripB, "biasC": np.ascontiguousarray(biasC), "x": np.ascontiguousarray(xcores[c]),
            "w_out": np.ascontiguousarray(w_out_l), "gout": np.ascontiguousarray(out_gain_l.reshape(16, 64).T),
            "g2": np.ascontiguousarray(norm_ffn_l.reshape(8, 128).T), "w_r": np.ascontiguousarray(w_r_l),
        })
    res = _run(kb, maps)
    return [r["xmid"] for r in res], [r["aff"] for r in res]


TG = 2048
NG = NTOK // TG
JB = 256
NJB = DEXP // JB
NBIS = 34


def build_l3(final):
    kb = KB()
    xmT = kb.dram("xmT", [D, NTOK], F32, "ExternalInput")
    xmid = kb.dram("xmid", [NTOK, D], F32, "ExternalInput")
    affl = kb.dram("affl", [NTOK, NEXP], F32, "ExternalInput")
    affP = kb.dram("affP", [128, SEQ_P // 8], F32, "ExternalInput")
    affS = kb.dram("affS", [128, NSEQ_S * SEQ_S // 8], F32, "ExternalInput")
    BDd = kb.dram("BD", [128, 128], F32, "ExternalInput")
    Seld = kb.dram("Sel", [128, NEXP], F32, "ExternalInput")
    g2d = kb.dram("g2", [128, 8], F32, "ExternalInput")
    gfd = kb.dram("gfin", [128, D], F32, "ExternalInput")
    wg = kb.dram("wg", [NEXP, D, DEXP], F32, "ExternalInput")
    wu = kb.dram("wu", [NEXP, D, DEXP], F32, "ExternalInput")
    wd = kb.dram("wd", [NEXP, DEXP, D], F32, "ExternalInput")
    xo = kb.dram("xo", [NTOK, D], F32, "ExternalOutput")

    big = kb.sb([128, 16384], F32, "big")
    AS = big.ap[:, 0:8192]
    MK = big.ap[:, 8192:12288].bitcast(BF16)
    APv = big.ap[:, 12288:14336]
    BD32 = kb.sb([128, 128], F32, "BD32")
    BD = kb.sb([128, 128], BF16, "BDb")
    Sel = kb.sb([128, NEXP], F32, "Selb")
    g2 = kb.sb([128, 8], F32, "g2sb")
    gf = kb.sb([128, D], F32, "gfsb")
    onesb = kb.sb([128, 128], BF16, "onesb")
    epsb = kb.sb([128, 1], F32, "epsb")
    AL = kb.sb([128, NT, NEXP], F32, "AL")
    GT = kb.sb([128, NT, NEXP], F32, "GT")
    small = {n: kb.sb([128, 1], F32, "sm_" + n) for n in ("lo", "hi", "md", "c1", "cl32", "ge", "nge", "t1", "t2", "rem", "pf")}
    chb = kb.sb([128, 1], BF16, "chb")
    clb = kb.sb([128, 1], BF16, "clb")
    pb = kb.sb([128, 1], BF16, "pb")
    R32 = kb.sb([128, NEXP], F32, "R32")
    Rb = [kb.sb([128, NEXP], BF16, f"Rb{i}") for i in range(3)]
    Tthr = [kb.sb([128, NEXP], F32, f"Tthr{i}") for i in range(2)]
    psm = kb.ps([128, 512], F32, "psm")
    kb.op("pool", lambda e: e.memset(onesb.ap, 1.0), writes=[onesb])
    kb.op("pool", lambda e: e.memset(epsb.ap, EPS), writes=[epsb])
    kb.dma("sp", BD32.ap, BDd, writes=[BD32])
    kb.op("dve", lambda e: e.tensor_copy(out=BD.ap, in_=BD32.ap), reads=[BD32], writes=[BD])
    kb.dma("sp", Sel.ap, Seld, writes=[Sel])
    kb.dma("sp", g2.ap, g2d, writes=[g2])
    kb.dma("sp", gf.ap, gfd, writes=[gf])
    kb.dma("sp", AL.ap, affl.rearrange("(t p) e -> p t e", p=128), writes=[AL])
    kb.dma("sp", AS, affS, writes=[big])
    kb.dma("sp", APv, affP, writes=[big])
    S = small

    def dve(fn, reads, writes):
        return kb.op("dve", fn, reads=reads, writes=writes)

    for gi, (Abuf, Aap, n, cap) in enumerate(((big, APv, SEQ_P // 8, SEQ_P // 8), (big, AS, NSEQ_S * SEQ_S // 8, NSEQ_S * SEQ_S // 8))):
        dve(lambda e: e.memset(S["lo"].ap, 0.0), [], [S["lo"]])
        dve(lambda e: e.memset(S["hi"].ap, 1.0), [], [S["hi"]])
        for it in range(NBIS):
            dve(lambda e: e.tensor_tensor(out=S["md"].ap, in0=S["lo"].ap, in1=S["hi"].ap, op=ALU.add), [S["lo"], S["hi"]], [S["md"]])
            dve(lambda e: e.tensor_scalar(out=S["md"].ap, in0=S["md"].ap, scalar1=0.5, scalar2=None, op0=ALU.mult), [S["md"]], [S["md"]])
            dve(lambda e: e.tensor_scalar(out=MK[:, 0:n], in0=Aap, scalar1=S["md"].ap[:, 0:1], scalar2=None, op0=ALU.is_ge), [Abuf, S["md"]], [big])
            dve(lambda e: e.tensor_reduce(out=S["c1"].ap, in_=MK[:, 0:n], axis=AX.X, op=ALU.add), [big], [S["c1"]])
            dve(lambda e: e.tensor_copy(out=chb.ap, in_=S["c1"].ap), [S["c1"]], [chb])
            dve(lambda e: e.tensor_tensor(out=clb.ap, in0=S["c1"].ap, in1=chb.ap, op=ALU.subtract), [S["c1"], chb], [clb])
            kb.op("pe", lambda e: e.matmul(psm.ap[:, 0:1], lhsT=BD.ap, rhs=chb.ap, start=True, stop=False), reads=[BD, chb], writes=[psm])
            kb.op("pe", lambda e: e.matmul(psm.ap[:, 0:1], lhsT=BD.ap, rhs=clb.ap, start=False, stop=True), reads=[BD, clb], writes=[psm])
            dve(lambda e: e.tensor_scalar(out=S["ge"].ap, in0=psm.ap[:, 0:1], scalar1=float(cap), scalar2=None, op0=ALU.is_ge), [psm], [S["ge"]])
            dve(lambda e: e.tensor_scalar(out=S["nge"].ap, in0=S["ge"].ap, scalar1=-1.0, scalar2=1.0, op0=ALU.mult, op1=ALU.add), [S["ge"]], [S["nge"]])
            dve(lambda e: e.tensor_tensor(out=S["t1"].ap, in0=S["lo"].ap, in1=S["nge"].ap, op=ALU.mult), [S["lo"], S["nge"]], [S["t1"]])
            dve(lambda e: e.scalar_tensor_tensor(out=S["lo"].ap, in0=S["md"].ap, scalar=S["ge"].ap[:, 0:1], in1=S["t1"].ap, op0=ALU.mult, op1=ALU.add),
                [S["md"], S["ge"], S["t1"]], [S["lo"]])
            dve(lambda e: e.tensor_tensor(out=S["t2"].ap, in0=S["hi"].ap, in1=S["ge"].ap, op=ALU.mult), [S["hi"], S["ge"]], [S["t2"]])
            dve(lambda e: e.scalar_tensor_tensor(out=S["hi"].ap, in0=S["md"].ap, scalar=S["nge"].ap[:, 0:1], in1=S["t2"].ap, op0=ALU.mult, op1=ALU.add),
                [S["md"], S["nge"], S["t2"]], [S["hi"]])
        dve(lambda e: e.tensor_copy(out=S["rem"].ap, in_=S["lo"].ap), [S["lo"]], [S["rem"]])
        for k in range(3):
            dve(lambda e: e.tensor_copy(out=pb.ap, in_=S["rem"].ap), [S["rem"]], [pb])
            dve(lambda e: e.tensor_copy(out=S["pf"].ap, in_=pb.ap), [pb], [S["pf"]])
            dve(lambda e: e.tensor_tensor(out=S["rem"].ap, in0=S["rem"].ap, in1=S["pf"].ap, op=ALU.subtract), [S["rem"], S["pf"]], [S["rem"]])
            dve(lambda e: e.tensor_scalar(out=R32.ap, in0=Sel.ap, scalar1=S["pf"].ap[:, 0:1], scalar2=None, op0=ALU.mult), [Sel, S["pf"]], [R32])
            dve(lambda e, k=k: e.tensor_copy(out=Rb[k].ap, in_=R32.ap), [R32], [Rb[k]])
        for k in range(3):
            kb.op("pe", lambda e, k=k: e.matmul(psm.ap[:, 16:32], lhsT=onesb.ap, rhs=Rb[k].ap, start=(k == 0), stop=(k == 2)), reads=[onesb, Rb[k]], writes=[psm])
        dve(lambda e, gi=gi: e.tensor_copy(out=Tthr[gi].ap, in_=psm.ap[:, 16:32]), [psm], [Tthr[gi]])
    for (t0, t1, gi) in ((0, 16, 0), (16, NT, 1)):
        nt_ = t1 - t0
        dve(lambda e, t0=t0, t1=t1, gi=gi, nt_=nt_: e.tensor_tensor(out=GT.ap[:, t0:t1, :], in0=AL.ap[:, t0:t1, :],
                                                                in1=Tthr[gi].ap.unsqueeze(1).to_broadcast([128, nt_, NEXP]), op=ALU.is_ge), [AL, Tthr[gi]], [GT])
        dve(lambda e, t0=t0, t1=t1: e.tensor_tensor(out=GT.ap[:, t0:t1, :], in0=GT.ap[:, t0:t1, :], in1=AL.ap[:, t0:t1, :], op=ALU.mult), [GT, AL], [GT])

    stg = [kb.sb([128, 8, JB], F32, f"stg{i}") for i in range(2)]
    Wg = [kb.sb([128, 8, JB], BF16, f"Wg{i}") for i in range(2)]
    Wu = [kb.sb([128, 8, JB], BF16, f"Wu{i}") for i in range(2)]
    Wd = [kb.sb([128, JB // 128, D], BF16, f"Wd{i}") for i in range(2)]
    hT = kb.sb([128, 8, TG], BF16, "hT")
    sq = kb.sb([128, 8, JB], BF16, "sq")
    rb = kb.sb([128, JB], F32, "rb")
    hid = [kb.sb([128, JB // 128, 512], BF16, f"hid{i}") for i in range(2)]
    sg = [kb.sb([128, 512], F32, f"sg{i}") for i in range(2)]
    fin = [kb.sb([128, D], F32, f"fin{i}") for i in range(2)]
    junk = kb.sb([128, D], BF16, "junk")
    fs = [kb.sb([128, 2], F32, f"fs{i}") for i in range(2)]
    gps = [kb.ps([128, 512], F32, f"gps{i}") for i in range(2)]
    ups = [kb.ps([128, 512], F32, f"ups{i}") for i in range(2)]
    yps = [kb.ps([128, 512], F32, f"yps{i}") for i in range(2)]
    XA = big.ap[:, 0:(TG // 128) * D].rearrange("p (t d) -> p t d", d=D)
    xT_v = xmT.rearrange("(c p) t -> p c t", p=128)
    wg_v = wg.rearrange("e (c p) j -> e p c j", p=128)
    wu_v = wu.rearrange("e (c p) j -> e p c j", p=128)
    wd_v = wd.rearrange("e (jc p) n -> e p jc n", p=128)
    ctr = {"s": 0, "w": 0, "g": 0, "y": 0, "h": 0}
    for G in range(NG):
        tok0 = G * TG
        kb.dma("sp", XA, xmid[tok0:tok0 + TG, :].rearrange("(t p) d -> p t d", p=128), writes=[big])
        for q in range(TG // JB):
            st = stg[ctr["s"] % 2]; ctr["s"] += 1
            kb.dma("sp", st.ap, xT_v[:, :, tok0 + q * JB:tok0 + (q + 1) * JB], writes=[st])
            kb.op("act", lambda e, st=st: e.activation(out=sq.ap, in_=st.ap, func=AF.Square), reads=[st], writes=[sq])
            for c in range(8):
                kb.op("pe", lambda e, c=c: e.matmul(psm.ap[:, 0:JB], lhsT=onesb.ap, rhs=sq.ap[:, c, :], start=(c == 0), stop=(c == 7)), reads=[onesb, sq], writes=[psm])
            kb.op("act", lambda e: e.activation(out=rb.ap, in_=psm.ap[:, 0:JB], func=AF.Sqrt, scale=1.0 / D, bias=epsb.ap[:, 0:1]), reads=[psm, epsb], writes=[rb])
            kb.op("dve", lambda e: e.reciprocal(out=rb.ap, in_=rb.ap), reads=[rb], writes=[rb])
            for c in range(8):
                kb.op("dve", lambda e, c=c, st=st, q=q: e.scalar_tensor_tensor(out=hT.ap[:, c, q * JB:(q + 1) * JB], in0=st.ap[:, c, :], scalar=g2.ap[:, c:c + 1],
                                                                           in1=rb.ap, op0=ALU.mult, op1=ALU.mult), reads=[st, g2, rb], writes=[hT])
        def load_w(ex, jb, w):
            for (src, dst) in ((wg_v[ex, :, :, jb * JB:(jb + 1) * JB], Wg[w]), (wu_v[ex, :, :, jb * JB:(jb + 1) * JB], Wu[w])):
                st = stg[ctr["s"] % 2]; ctr["s"] += 1
                kb.dma("sp", st.ap, src, writes=[st])
                kb.op("pool", lambda e, st=st, dst=dst: e.tensor_copy(out=dst.ap, in_=st.ap), reads=[st], writes=[dst])
            st = stg[ctr["s"] % 2]; ctr["s"] += 1
            stv = st.ap.rearrange("p c j -> p (c j)").rearrange("p (jc n) -> p jc n", n=D)
            kb.dma("sp", stv, wd_v[ex, :, jb * (JB // 128):(jb + 1) * (JB // 128), :], writes=[st])
            kb.op("pool", lambda e, stv=stv, w=w: e.tensor_copy(out=Wd[w].ap, in_=stv), reads=[st], writes=[Wd[w]])

        def up(ex, jb, w, ck, H):
            for jc in range(JB // 128):
                gp = gps[ctr["g"] % 2]; up_ = ups[ctr["g"] % 2]; SG = sg[ctr["g"] % 2]; ctr["g"] += 1
                for c in range(8):
                    kb.op("pe", lambda e, c=c, gp=gp, jc=jc: e.matmul(gp.ap, lhsT=Wg[w].ap[:, c, jc * 128:(jc + 1) * 128], rhs=hT.ap[:, c, ck * 512:(ck + 1) * 512],
                                                                 start=(c == 0), stop=(c == 7)), reads=[Wg[w], hT], writes=[gp])
                for c in range(8):
                    kb.op("pe", lambda e, c=c, up_=up_, jc=jc: e.matmul(up_.ap, lhsT=Wu[w].ap[:, c, jc * 128:(jc + 1) * 128], rhs=hT.ap[:, c, ck * 512:(ck + 1) * 512],
                                                                   start=(c == 0), stop=(c == 7)), reads=[Wu[w], hT], writes=[up_])
                kb.op("act", lambda e, gp=gp, SG=SG: e.activation(out=SG.ap, in_=gp.ap, func=AF.Silu), reads=[gp], writes=[SG])
                kb.op("dve", lambda e, up_=up_, SG=SG, H=H, jc=jc: e.tensor_tensor(out=H.ap[:, jc, :], in0=up_.ap, in1=SG.ap, op=ALU.mult), reads=[up_, SG], writes=[H])

        def down(ex, jb, w, ck, H):
            for tt in range(4):
                tile = ck * 4 + tt
                gtile = G * (TG // 128) + tile
                for half in range(2):
                    yp = yps[ctr["y"] % 2]; ctr["y"] += 1
                    for jc in range(JB // 128):
                        kb.op("pe", lambda e, jc=jc, yp=yp, half=half, tt=tt: e.matmul(yp.ap, lhsT=H.ap[:, jc, tt * 128:(tt + 1) * 128], rhs=Wd[w].ap[:, jc, half * 512:(half + 1) * 512],
                                                                                  start=(jc == 0), stop=(jc == JB // 128 - 1)), reads=[H, Wd[w]], writes=[yp])
                    kb.op("dve", lambda e, yp=yp, half=half, tile=tile, gtile=gtile: e.scalar_tensor_tensor(
                        out=XA[:, tile, half * 512:(half + 1) * 512], in0=yp.ap, scalar=GT.ap[:, gtile, ex:ex + 1],
                        in1=XA[:, tile, half * 512:(half + 1) * 512], op0=ALU.mult, op1=ALU.add), reads=[yp, GT, big], writes=[big])

        items = []
        for ex in range(NEXP):
            for jb in range(NJB):
                w = ctr["w"] % 2; ctr["w"] += 1
                for ck in range(TG // 512):
                    items.append((ex, jb, w, ck))
        prev = None
        for it in items + [None]:
            if it is not None:
                ex, jb, w, ck = it
                if ck == 0:
                    load_w(ex, jb, w)
                H = hid[ctr["h"] % 2]; ctr["h"] += 1
                up(ex, jb, w, ck, H)
                cur = (ex, jb, w, ck, H)
            else:
                cur = None
            if prev is not None:
                down(*prev)
            prev = cur
        for tile in range(TG // 128):
            r0 = tok0 + tile * 128
            if not final:
                kb.dma("pool", xo[r0:r0 + 128, :], XA[:, tile, :], reads=[big], is_output=True)
            else:
                F_, FS = fin[tile % 2], fs[tile % 2]
                kb.op("act", lambda e, tile=tile, FS=FS: e.activation(out=junk.ap, in_=XA[:, tile, :], func=AF.Square, accum_out=FS.ap[:, 0:1]), reads=[big], writes=[junk, FS])
                kb.op("act", lambda e, FS=FS: e.activation(out=FS.ap[:, 0:1], in_=FS.ap[:, 0:1], func=AF.Sqrt, scale=1.0 / D, bias=epsb.ap[:, 0:1]), reads=[FS, epsb], writes=[FS])
                kb.op("dve", lambda e, FS=FS: e.reciprocal(out=FS.ap[:, 0:1], in_=FS.ap[:, 0:1]), reads=[FS], writes=[FS])
                kb.op("dve", lambda e, tile=tile, F_=F_, FS=FS: e.scalar_tensor_tensor(out=F_.ap, in0=XA[:, tile, :], scalar=FS.ap[:, 0:1], in1=gf.ap, op0=ALU.mult, op1=ALU.mult),
                      reads=[big, FS, gf], writes=[F_])
                kb.dma("pool", xo[r0:r0 + 128, :], F_.ap, reads=[F_], is_output=True)
    return kb


def run_l3(xmids, affs, norm_ffn_l, wg_l, wu_l, wd_l, final_norm, final):
    kb = build_l3(final)
    affP = np.concatenate([a[:SEG] for a in affs], 0)
    affS = np.concatenate([a[SEG:] for a in affs], 0)

    def lay(a):
        n = a.shape[0]
        return np.ascontiguousarray(a.T.reshape(NEXP, 8, n // 8).reshape(128, n // 8))

    p = np.arange(128)
    BD = (p[:, None] // 8 == p[None, :] // 8).astype(np.float32)
    Sel = (p[:, None] == 8 * np.arange(NEXP)[None, :]).astype(np.float32)
    base = {"affP": lay(affP), "affS": lay(affS), "BD": BD, "Sel": Sel,
            "g2": np.ascontiguousarray(norm_ffn_l.reshape(8, 128).T),
            "gfin": np.ascontiguousarray(np.broadcast_to(final_norm, (128, D))),
            "wg": np.ascontiguousarray(wg_l), "wu": np.ascontiguousarray(wu_l), "wd": np.ascontiguousarray(wd_l)}
    maps = []
    for c in range(NCORES):
        m = dict(base)
        m["xmT"] = np.ascontiguousarray(xmids[c].T)
        m["xmid"] = np.ascontiguousarray(xmids[c])
        m["affl"] = np.ascontiguousarray(affs[c])
        maps.append(m)
    res = _run(kb, maps)
    return [r["xo"] for r in res]


def kernel(x_prompt, x_sample, w_in, w_out, norm_mix, norm_ffn, q_gain, k_gain, out_gain, na_rpb, rel_bias,
           w_router, w_gate, w_up, w_down, final_norm):
    f = lambda a: np.asarray(a, dtype=np.float32)
    xp, xs = f(x_prompt)[0], f(x_sample)
    w_in, w_out, norm_mix, norm_ffn, q_gain, k_gain, out_gain = map(f, (w_in, w_out, norm_mix, norm_ffn, q_gain, k_gain, out_gain))
    na_rpb, rel_bias, w_router, w_gate, w_up, w_down, final_norm = map(f, (na_rpb, rel_bias, w_router, w_gate, w_up, w_down, final_norm))
    xc = [core_tokens(xp, xs, c) for c in range(NCORES)]
    for l in range(DEPTH):
        projs = run_l1(xc, w_in[l], norm_mix[l], q_gain[l], k_gain[l])
        xm, aff = run_l2(projs, xc, w_out[l], out_gain[l], norm_ffn[l], w_router[l], rel_bias, na_rpb[l])
        xc = run_l3(xm, aff, norm_ffn[l], w_gate[l], w_up[l], w_down[l], final_norm, final=(l == DEPTH - 1))
    y_prompt = np.concatenate([xc[c][:SEG] for c in range(NCORES)], 0)[None]
    y_sample = np.stack([xc[s // 4][SEG * (1 + s % 4):SEG * (2 + s % 4)] for s in range(NSEQ_S)], 0)
    return (np.ascontiguousarray(y_prompt, dtype=np.float32), np.ascontiguousarray(y_sample, dtype=np.float32))
```
